# Optimizing a Trainium2 kernel written in Bass

```python
import math
import jax, jax.numpy as jnp
from jax import lax
import numpy as np

D_MODEL = 1024
BATCH = 8
SEQ = 4096
DEPTH = 2

N_MIXERS = 2
POOL_WINDOWS = (2, 4, 8, 16)
POOL_GROUPS = len(POOL_WINDOWS)
POOL_GROUP_DIM = D_MODEL // POOL_GROUPS
N_HEADS = 16
N_KV_GROUPS = 4
HEADS_PER_GROUP = N_HEADS // N_KV_GROUPS
HEAD_DIM = D_MODEL // N_HEADS
Q_DIM = N_HEADS * HEAD_DIM
KV_DIM = N_KV_GROUPS * HEAD_DIM
N_BRANCHES = 3
IN_PROJ_DIM = Q_DIM + 2 * N_BRANCHES * KV_DIM + N_BRANCHES * N_HEADS
CMP_STRIDE = 16
CMP_BLOCK = 2 * CMP_STRIDE
CMP_HIDDEN = 2 * HEAD_DIM
SEL_BLOCK = 64
SEL_TOP_N = 16
WINDOW = 512
Q_BLOCK = 64
FORCE_BONUS = 1.0e3
NEG_INF = -1.0e30
ROPE_THETA = 10000.0
ATTN_SCALE = HEAD_DIM ** -0.5
D_FF = 2816
N_EXPERTS = 8
TOP_K = 2
D_FF_EXPERT = 3584
LN_EPS = 1e-5
ALPHA = (2 * DEPTH) ** 0.25
BETA = (8 * DEPTH) ** -0.25

kernel_name = "hybrid_pool_nsa_moe_deepnorm"


def layer_norm(x, g, b):
    xf = x.astype(jnp.float32)
    mu = jnp.mean(xf, axis=-1, keepdims=True)
    var = jnp.mean(jnp.square(xf - mu), axis=-1, keepdims=True)
    return ((xf - mu) * lax.rsqrt(var + LN_EPS) * g.astype(jnp.float32) + b.astype(jnp.float32)).astype(x.dtype)


def rope(x, pos):
    half = x.shape[-1] // 2
    freqs = jnp.power(ROPE_THETA, -jnp.arange(half, dtype=jnp.float32) / half)
    ang = pos.astype(jnp.float32)[:, None] * freqs[None, :]
    cos = jnp.cos(ang)[None, :, None, :]
    sin = jnp.sin(ang)[None, :, None, :]
    xf = x.astype(jnp.float32)
    x1, x2 = xf[..., :half], xf[..., half:]
    return jnp.concatenate([x1 * cos - x2 * sin, x1 * sin + x2 * cos], axis=-1).astype(x.dtype)


def pool_mixer(x, w_grp, scale):
    B, S, _ = x.shape
    xg = x.reshape(B, S, POOL_GROUPS, POOL_GROUP_DIM)
    c = jnp.concatenate([jnp.zeros((B, 1, POOL_GROUPS, POOL_GROUP_DIM), jnp.float32),
                         jnp.cumsum(xg.astype(jnp.float32), axis=1)], axis=1)
    t1 = jnp.arange(1, S + 1)
    pooled = []
    for gi, w in enumerate(POOL_WINDOWS):
        cg = c[:, :, gi]
        lo = cg[:, jnp.maximum(t1 - w, 0)]
        cnt = jnp.minimum(t1, w).astype(jnp.float32)
        pooled.append((cg[:, 1:] - lo) / cnt[None, :, None])
    pooled = jnp.stack(pooled, axis=2).astype(x.dtype)
    y = jnp.einsum('bsgc,gcd->bsgd', pooled - xg, w_grp).reshape(B, S, D_MODEL)
    return y * scale


def compress(k, pe, w1, w2):
    B, S, G, D = k.shape
    ch = k.reshape(B, S // CMP_STRIDE, CMP_STRIDE, G, D)
    blk = jnp.concatenate([ch[:, :-1], ch[:, 1:]], axis=2)
    blk = blk + pe[None, None, :, None, :]
    nc = blk.shape[1]
    blk = blk.transpose(0, 1, 3, 2, 4).reshape(B, nc, G, CMP_BLOCK * D)
    return jax.nn.silu(blk @ w1) @ w2


def nsa_mixer(x, w_in, pe_k, w1_k, w2_k, pe_v, w1_v, w2_v, w_out):
    B, S, _ = x.shape
    G, HG, D = N_KV_GROUPS, HEADS_PER_GROUP, HEAD_DIM
    proj = x @ w_in
    splits = [Q_DIM + i * KV_DIM for i in range(2 * N_BRANCHES + 1)]
    q, kc_raw, vc_raw, ks, vs, kw, vw, g_logits = jnp.split(proj, splits, axis=-1)
    pos = jnp.arange(S)
    q = rope(q.reshape(B, S, N_HEADS, D), pos)
    q = q.reshape(B, S, G, HG, D).transpose(0, 2, 3, 1, 4)
    kv = lambda a: a.reshape(B, S, G, D)
    ks = rope(kv(ks), pos)
    kw = rope(kv(kw), pos)
    vs, vw = kv(vs), kv(vw)
    kc = compress(kv(kc_raw), pe_k, w1_k, w2_k)
    vc = compress(kv(vc_raw), pe_v, w1_v, w2_v)
    nc = kc.shape[1]
    cend = CMP_STRIDE * jnp.arange(nc) + CMP_BLOCK - 1
    kc = rope(kc, cend)
    gates = jax.nn.sigmoid(g_logits.astype(jnp.float32)).astype(x.dtype).reshape(B, S, N_HEADS, N_BRANCHES)

    nsel = S // SEL_BLOCK
    n_top = min(SEL_TOP_N, nsel)
    ks_blk = ks.reshape(B, nsel, SEL_BLOCK, G, D).transpose(0, 3, 1, 2, 4)
    vs_blk = vs.reshape(B, nsel, SEL_BLOCK, G, D).transpose(0, 3, 1, 2, 4)
    kw_pad = jnp.pad(kw, ((0, 0), (WINDOW, 0), (0, 0), (0, 0)))
    vw_pad = jnp.pad(vw, ((0, 0), (WINDOW, 0), (0, 0), (0, 0)))
    cstart = CMP_STRIDE * jnp.arange(nc)
    sstart = SEL_BLOCK * jnp.arange(nsel)
    overlap = ((cstart[:, None] <= sstart[None, :] + SEL_BLOCK - 1) &
               (cstart[:, None] + CMP_BLOCK - 1 >= sstart[None, :])).astype(jnp.float32)
    bi = jnp.arange(B)[:, None, None, None]
    gi = jnp.arange(G)[None, :, None, None]
    jsel = jnp.arange(nsel)

    def query_block(i):
        t0 = i * Q_BLOCK
        t = t0 + jnp.arange(Q_BLOCK)
        qb = lax.dynamic_slice_in_dim(q, t0, Q_BLOCK, axis=3)
        sc = jnp.einsum('bghtd,bngd->bghtn', qb, kc).astype(jnp.float32) * ATTN_SCALE
        cvalid = cend[None, :] <= t[:, None]
        pc = jax.nn.softmax(jnp.where(cvalid, sc, NEG_INF), axis=-1)
        pc = pc * jnp.any(cvalid, axis=-1).astype(jnp.float32)[:, None]
        o_c = jnp.einsum('bghtn,bngd->btghd', pc.astype(vc.dtype), vc)
        imp = jnp.einsum('bghtn,nj->bgtj', pc, overlap)
        blk_t = t // SEL_BLOCK
        bvalid = jsel[None, :] <= blk_t[:, None]
        forced = (jsel[None, :] == 0) | (jsel[None, :] == blk_t[:, None]) | (jsel[None, :] == blk_t[:, None] - 1)
        score = jnp.where(bvalid, imp + FORCE_BONUS * forced.astype(jnp.float32), -1.0)
        _, idx = lax.top_k(score, n_top)
        sel_valid = idx <= blk_t[None, None, :, None]
        kg = ks_blk[bi, gi, idx]
        vg = vs_blk[bi, gi, idx]
        ss = jnp.einsum('bghtd,bgtnld->bghtnl', qb, kg).astype(jnp.float32) * ATTN_SCALE
        kpos = idx[..., None] * SEL_BLOCK + jnp.arange(SEL_BLOCK)
        ms = (kpos <= t[None, None, :, None, None]) & sel_valid[..., None]
        ss = jnp.where(ms[:, :, None], ss, NEG_INF)
        ps = jax.nn.softmax(ss.reshape(ss.shape[:4] + (n_top * SEL_BLOCK,)), axis=-1).reshape(ss.shape)
        o_s = jnp.einsum('bghtnl,bgtnld->btghd', ps.astype(vg.dtype), vg)
        kwb = lax.dynamic_slice_in_dim(kw_pad, t0, Q_BLOCK + WINDOW, axis=1)
        vwb = lax.dynamic_slice_in_dim(vw_pad, t0, Q_BLOCK + WINDOW, axis=1)
        kpos_w = t0 - WINDOW + jnp.arange(Q_BLOCK + WINDOW)
        mw = (kpos_w[None, :] <= t[:, None]) & (kpos_w[None, :] > t[:, None] - WINDOW) & (kpos_w[None, :] >= 0)
        sw = jnp.einsum('bghtd,bkgd->bghtk', qb, kwb).astype(jnp.float32) * ATTN_SCALE
        pw = jax.nn.softmax(jnp.where(mw, sw, NEG_INF), axis=-1)
        o_w = jnp.einsum('bghtk,bkgd->btghd', pw.astype(vwb.dtype), vwb)
        gb = lax.dynamic_slice_in_dim(gates, t0, Q_BLOCK, axis=1).reshape(B, Q_BLOCK, G, HG, N_BRANCHES)
        return gb[..., 0:1] * o_c + gb[..., 1:2] * o_s + gb[..., 2:3] * o_w

    out = lax.map(query_block, jnp.arange(S // Q_BLOCK))
    out = out.transpose(1, 0, 2, 3, 4, 5).reshape(B, S, Q_DIM)
    return out @ w_out


def swiglu(x, w_gu, w_down):
    g, u = jnp.split(x @ w_gu, 2, axis=-1)
    return (jax.nn.silu(g) * u) @ w_down


def moe_ffn(x, router, w_gu, w_down):
    B, S, D = x.shape
    xt = x.reshape(B * S, D)
    logits = (xt @ router).astype(jnp.float32)
    top_v, top_i = lax.top_k(logits, TOP_K)
    w = jax.nn.softmax(top_v, axis=-1)
    combine = jnp.einsum('tk,tke->te', w, jax.nn.one_hot(top_i, N_EXPERTS, dtype=jnp.float32)).astype(x.dtype)
    y = jnp.zeros_like(xt)
    for e in range(N_EXPERTS):
        y = y + combine[:, e:e + 1] * swiglu(xt, w_gu[e], w_down[e])
    return y.reshape(B, S, D)


def setup_inputs(seed: int = 0) -> dict:
    key = jax.random.key(seed)
    keys = iter(jax.random.split(key, 32))
    nrm = lambda shape, s: jax.random.normal(next(keys), shape, jnp.float32) * s
    na = (DEPTH + 1) // 2
    nb = DEPTH // 2
    return {
        "x": nrm((BATCH, SEQ, D_MODEL), 1.0),
        "ln_g": 1.0 + nrm((DEPTH, 2, D_MODEL), 0.05),
        "ln_b": nrm((DEPTH, 2, D_MODEL), 0.02),
        "pool_w": nrm((na, POOL_GROUPS, POOL_GROUP_DIM, POOL_GROUP_DIM), BETA * POOL_GROUP_DIM ** -0.5),
        "pool_scale": 1.0 + nrm((na, D_MODEL), 0.1),
        "nsa_w_in": nrm((nb, D_MODEL, IN_PROJ_DIM), D_MODEL ** -0.5),
        "nsa_pe_k": nrm((nb, CMP_BLOCK, HEAD_DIM), 0.1),
        "nsa_w1_k": nrm((nb, CMP_BLOCK * HEAD_DIM, CMP_HIDDEN), (CMP_BLOCK * HEAD_DIM) ** -0.5),
        "nsa_w2_k": nrm((nb, CMP_HIDDEN, HEAD_DIM), CMP_HIDDEN ** -0.5),
        "nsa_pe_v": nrm((nb, CMP_BLOCK, HEAD_DIM), 0.1),
        "nsa_w1_v": nrm((nb, CMP_BLOCK * HEAD_DIM, CMP_HIDDEN), (CMP_BLOCK * HEAD_DIM) ** -0.5),
        "nsa_w2_v": nrm((nb, CMP_HIDDEN, HEAD_DIM), CMP_HIDDEN ** -0.5),
        "nsa_w_out": nrm((nb, Q_DIM, D_MODEL), BETA * Q_DIM ** -0.5),
        "ffn_w_gu": nrm((na, D_MODEL, 2 * D_FF), D_MODEL ** -0.5),
        "ffn_w_down": nrm((na, D_FF, D_MODEL), BETA * D_FF ** -0.5),
        "moe_router": nrm((nb, D_MODEL, N_EXPERTS), D_MODEL ** -0.5),
        "moe_w_gu": nrm((nb, N_EXPERTS, D_MODEL, 2 * D_FF_EXPERT), D_MODEL ** -0.5),
        "moe_w_down": nrm((nb, N_EXPERTS, D_FF_EXPERT, D_MODEL), BETA * D_FF_EXPERT ** -0.5),
    }


def reference(x, ln_g, ln_b, pool_w, pool_scale, nsa_w_in, nsa_pe_k, nsa_w1_k, nsa_w2_k,
              nsa_pe_v, nsa_w1_v, nsa_w2_v, nsa_w_out, ffn_w_gu, ffn_w_down,
              moe_router, moe_w_gu, moe_w_down):
    for i in range(DEPTH):
        j = i // N_MIXERS
        if i % N_MIXERS == 0:
            h = pool_mixer(x, pool_w[j], pool_scale[j])
        else:
            h = nsa_mixer(x, nsa_w_in[j], nsa_pe_k[j], nsa_w1_k[j], nsa_w2_k[j],
                          nsa_pe_v[j], nsa_w1_v[j], nsa_w2_v[j], nsa_w_out[j])
        x = layer_norm(ALPHA * x + h, ln_g[i, 0], ln_b[i, 0])
        if i % 2 == 0:
            f = swiglu(x, ffn_w_gu[j], ffn_w_down[j])
        else:
            f = moe_ffn(x, moe_router[j], moe_w_gu[j], moe_w_down[j])
        x = layer_norm(ALPHA * x + f, ln_g[i, 1], ln_b[i, 1])
    return x
```

```python
import contextlib
import numpy as np
import concourse.bass as bass
import concourse.mybir as mybir
from concourse.bass_utils import run_bass_kernel_spmd

F32 = mybir.dt.float32
BF16 = mybir.dt.bfloat16
I32 = mybir.dt.int32
AF = mybir.ActivationFunctionType
ALU = mybir.AluOpType

S = 4096
D = 1024
NT = S // 128
ALPHA = 4.0 ** 0.25
LN_EPS = 1e-5
D_FF = 2816
NEG = -30000.0
ATTN_SCALE = 0.125
NEXP = 8
DFE = 3584
CAP = 1280
IN_PROJ = 2608


class Prog:
    ENG = ['pe', 'act', 'dve', 'pool', 'sp']

    def __init__(self, nc):
        self.nc = nc
        self.streams = {e: [] for e in self.ENG}
        self.lastw = {}
        self.readers = {}
        self.dma_keys = {}

    def _add(self, eng, fn, reads, writes, dma_key=None):
        st = self.streams[eng]
        idx = len(st)
        me = (eng, idx)
        deps = set()
        for k in reads:
            w = self.lastw.get(k)
            if w is not None:
                deps.add(w)
        for k in writes:
            w = self.lastw.get(k)
            if w is not None:
                deps.add(w)
            for r in self.readers.get(k, {}).values():
                deps.add(r)
        deps.discard(me)
        rec = dict(fn=fn, deps=deps, dma_key=dma_key, signaled=False, eng=eng, idx=idx)
        if dma_key is not None:
            n = self.dma_keys.get(dma_key, 0) + 1
            self.dma_keys[dma_key] = n
            rec['dma_cnt'] = n
        st.append(rec)
        for k in reads:
            self.readers.setdefault(k, {})[eng if dma_key is None else (eng, dma_key)] = me
        for k in writes:
            self.lastw[k] = me
            self.readers[k] = {}
        return me

    def op(self, eng, fn, reads=(), writes=()):
        return self._add(eng, fn, list(reads), list(writes))

    def dma(self, eng, fn, reads=(), writes=(), key=None, count=1, raw=False):
        assert key is not None
        me = self._add(eng, fn, list(reads), list(writes), dma_key=key)
        rec = self.streams[eng][me[1]]
        rec['raw'] = raw
        if count > 1:
            self.dma_keys[key] += count - 1
            rec['dma_cnt'] += count - 1
        return me

    def barrier(self):
        tails = set()
        for e in self.ENG:
            st = self.streams[e]
            for r in reversed(st):
                if r['fn'] is not None and r['dma_key'] is None:
                    tails.add((e, r['idx']))
                    break
        lastdma = {}
        for e in self.ENG:
            for r in self.streams[e]:
                if r['dma_key'] is not None:
                    lastdma[r['dma_key']] = (e, r['idx'])
        alld = tails | set(lastdma.values())
        for e in self.ENG:
            self.streams[e].append(dict(fn=None, deps=set(alld), dma_key=None, signaled=False,
                                        eng=e, idx=len(self.streams[e])))
        self.lastw = {}
        self.readers = {}

    def emit(self):
        nc = self.nc
        for e in self.ENG:
            for r in self.streams[e]:
                drop = set()
                for (de, di) in r['deps']:
                    d = self.streams[de][di]
                    if d['fn'] is None:
                        drop.add((de, di))
                        continue
                    if de == 'pe' and e == 'pe' and d['dma_key'] is None and r['fn'] is not None:
                        drop.add((de, di))
                        continue
                    d['signaled'] = True
                r['deps'] -= drop
        for e in self.ENG:
            c = 0
            for r in self.streams[e]:
                if r['dma_key'] is None and r['signaled']:
                    c += 1
                    r['cnt'] = c
        with contextlib.ExitStack() as es:
            esem = {e: es.enter_context(nc.semaphore('s_' + e)) for e in self.ENG}
            dsem = {}
            for k in self.dma_keys:
                dsem[k] = es.enter_context(nc.semaphore('d_%d' % len(dsem)))
            block = es.enter_context(nc.Block())
            engobj = {'pe': 'tensor', 'act': 'scalar', 'dve': 'vector', 'pool': 'gpsimd', 'sp': 'sync'}

            def make(e):
                def body(eng):
                    seen = {}
                    for r in self.streams[e]:
                        for (de, di) in sorted(r['deps']):
                            d = self.streams[de][di]
                            if d['dma_key'] is not None:
                                sem = dsem[d['dma_key']]
                                val = 16 * d['dma_cnt']
                            else:
                                sem = esem[de]
                                val = d['cnt']
                            key = id(sem)
                            if seen.get(key, 0) >= val:
                                continue
                            seen[key] = val
                            eng.wait_ge(sem, val)
                        if r['fn'] is None:
                            continue
                        if r.get('raw'):
                            r['fn'](eng, dsem[r['dma_key']])
                            continue
                        ins = r['fn'](eng)
                        if r['dma_key'] is not None:
                            ins.then_inc(dsem[r['dma_key']], 16)
                        elif r['signaled']:
                            ins.then_inc(esem[e], 1)
                return body
            for e in self.ENG:
                getattr(block, engobj[e])(make(e))


class Arena:
    def __init__(self, big, ncols):
        self.big = big
        self.ncols = ncols
        self.off = 0
        self.mark_ = 0

    def f32(self, cols):
        a = self.off
        self.off += cols
        assert self.off <= self.ncols, ("SBUF arena overflow", self.off, self.ncols)
        return self.big[:, a:a + cols]

    def bf16(self, cols):
        c4 = (cols + 1) // 2
        return self.f32(c4).bitcast(BF16)[:, 0:cols]

    def i32(self, cols):
        return self.f32(cols).bitcast(I32)

    def mark(self):
        self.mark_ = self.off

    def reset(self):
        self.off = self.mark_


class K:
    def __init__(self, nc, phases, ext):
        self.nc = nc
        self.P = Prog(nc)
        self.phases = phases
        self.ext = ext
        self.dram = {}
        self.rr = 0
        self.debug = False
        self.dbg_outs = []

    def din(self, name, shape, dt=F32):
        t = self.nc.dram_tensor(name, list(shape), dt, kind="ExternalInput")
        self.dram[name] = t.ap()
        return self.dram[name]

    def dmid(self, name, shape, dt, produced_here, consumed_here):
        if produced_here and consumed_here:
            t = self.nc.dram_tensor(name, list(shape), dt)
        elif produced_here:
            t = self.nc.dram_tensor(name, list(shape), dt, kind="ExternalOutput")
        else:
            t = self.nc.dram_tensor(name, list(shape), dt, kind="ExternalInput")
        self.dram[name] = t.ap()
        return self.dram[name]

    def dbg(self, name, ap, shape, dt, reads):
        if not getattr(self, 'debug', False):
            return
        t = self.nc.dram_tensor(name, list(shape), dt, kind="ExternalOutput")
        self.dbg_outs.append(name)
        self.load('sp', t.ap(), ap, reads=reads, writes=[('dbg', name)], key='dbg')

    def load(self, eng, out, in_, reads=(), writes=(), key=None):
        cg = getattr(self, '_cg', None)
        if cg is not None and isinstance(key, str) and key.startswith('c_') or (cg is not None and isinstance(key, tuple) and isinstance(key[0], str) and key[0].startswith('c_')):
            me = self.P.dma(eng, lambda e: e.dma_start(out=out, in_=in_), reads=reads, writes=writes, key=cg['key'])
            cg['last'] = me
            cg['writes'].extend(list(writes))
            return
        self.P.dma(eng, lambda e: e.dma_start(out=out, in_=in_), reads=reads, writes=writes, key=key)

    def cg_begin(self, key):
        self._cg = dict(key=key, last=None, writes=[])

    def cg_end(self):
        cg = self._cg
        self._cg = None
        if cg['last'] is not None:
            for w in cg['writes']:
                self.P.lastw[w] = cg['last']

    def evac_eng(self):
        self.rr += 1
        return 'act' if self.rr % 2 else 'dve'

    def copy(self, eng, out, in_, reads, writes):
        if eng == 'act':
            self.P.op('act', lambda e: e.activation(out=out, in_=in_, func=AF.Copy), reads=reads, writes=writes)
        else:
            self.P.op(eng, lambda e: e.tensor_copy(out=out, in_=in_), reads=reads, writes=writes)

    def mm(self, out, lhsT, rhs, start, stop, reads, writes):
        self.P.op('pe', lambda e: e.matmul(out, lhsT=lhsT, rhs=rhs, start=start, stop=stop), reads=reads, writes=writes)

    def layernorm(self, z, zk, gbc, bbc, xo, xok, tmp, tk, mul_eng='pool'):
        self.ln1(z, zk, tmp, tk)
        self.ln2(z, zk, gbc, bbc, xo, xok, tmp, tk, mul_eng)

    def ln1(self, z, zk, tmp, tk):
        P = self.P
        st, mv, rs, nmr = tmp['st'], tmp['mv'], tmp['rs'], tmp['nmr']
        P.op('dve', lambda e: e.bn_stats(out=st[:, 0:6], in_=z[:, 0:512]), reads=[zk], writes=[tk + 'st0'])
        P.op('dve', lambda e: e.bn_stats(out=st[:, 6:12], in_=z[:, 512:1024]), reads=[zk], writes=[tk + 'st1'])
        P.op('dve', lambda e: e.bn_aggr(out=mv[:, 0:2], in_=st[:, 0:12].rearrange("p (a b) -> p a b", a=2)),
             reads=[tk + 'st0', tk + 'st1'], writes=[tk + 'mv'])
        P.op('act', lambda e: e.activation(out=rs[:, 0:1], in_=mv[:, 1:2], func=AF.Sqrt, bias=LN_EPS, scale=1.0),
             reads=[tk + 'mv'], writes=[tk + 'rs'])

    def ln2(self, z, zk, gbc, bbc, xo, xok, tmp, tk, mul_eng='pool'):
        self.ln2a(z, zk, tmp, tk)
        self.ln2b(gbc, bbc, xo, xok, tmp, tk, mul_eng)

    def ln2a(self, z, zk, tmp, tk):
        P = self.P
        st, mv, rs, nmr = tmp['st'], tmp['mv'], tmp['rs'], tmp['nmr']
        P.op('dve', lambda e: e.reciprocal(out=rs[:, 0:1], in_=rs[:, 0:1]), reads=[tk + 'rs'], writes=[tk + 'rs'])
        P.op('dve', lambda e: e.scalar_tensor_tensor(out=nmr[:, 0:1], in0=mv[:, 0:1], scalar=-1.0, in1=rs[:, 0:1],
                                                      op0=ALU.mult, op1=ALU.mult), reads=[tk + 'mv', tk + 'rs'], writes=[tk + 'nmr'])
        xn = tmp['xn']
        P.op('act', lambda e: e.activation(out=xn, in_=z, func=AF.Identity, bias=nmr[:, 0:1], scale=rs[:, 0:1]),
             reads=[zk, tk + 'rs', tk + 'nmr'], writes=[tk + 'xn'])

    def ln2b(self, gbc, bbc, xo, xok, tmp, tk, mul_eng='pool'):
        P = self.P
        xn = tmp['xn']
        P.op(mul_eng, lambda e: e.tensor_tensor(out=xn, in0=xn, in1=gbc, op=ALU.mult), reads=[tk + 'xn'], writes=[tk + 'xn'])
        P.op('dve', lambda e: e.tensor_tensor(out=xo, in0=xn, in1=bbc, op=ALU.add), reads=[tk + 'xn'], writes=[xok])

    def phase_A1(self, A, ps):
        P, nc, dr = self.P, self.nc, self.dram
        A.reset()
        xT_d, x_d = dr['xT'], dr['x']
        x1_d, x1T_d = dr['x1_d'], dr['x1T_d']
        xh = [A.f32(8 * 528).rearrange("p (c t) -> p c t", c=8) for _ in range(2)]
        sA = A.f32(2 * 528).rearrange("p (c t) -> p c t", c=2)
        sB = A.f32(2 * 528).rearrange("p (c t) -> p c t", c=2)
        diffT = [A.bf16(8 * 512).rearrange("p (c t) -> p c t", c=8) for _ in range(2)]
        poolw = A.bf16(4 * 2 * 256).rearrange("p (g k d) -> p g k d", g=4, k=2)
        scale_bc, g_bc, b_bc = A.f32(1024), A.f32(1024), A.f32(1024)
        invc = A.f32(4 * 512).rearrange("p (g t) -> p g t", g=4)
        ident = A.bf16(128)
        NQ = 3
        xtok = [A.f32(1024) for _ in range(NQ)]
        hs = [A.f32(1024) for _ in range(NQ)]
        z = [A.f32(1024) for _ in range(NQ)]
        xn = [A.f32(1024) for _ in range(NQ)]
        xo = [A.f32(1024) for _ in range(NQ)]
        xb = [A.bf16(1024) for _ in range(NQ)]
        xst = [A.bf16(8 * 512).rearrange("p (c t) -> p c t", c=8) for _ in range(2)]
        small = [dict(st=A.f32(12), mv=A.f32(2), rs=A.f32(1), nmr=A.f32(1)) for _ in range(NQ)]
        self.cg_begin('cgA1')
        self.load('pool', poolw, dr['pool_w'].rearrange("g (k p) d -> p g k d", p=128), writes=['poolw'], key='c_poolw')
        self.load('sp', scale_bc, dr['pool_scale'][0].partition_broadcast(128), writes=['scale_bc'], key='c_scale')
        self.load('sp', g_bc, dr['ln_g'][0, 0].partition_broadcast(128), writes=['g_bc'], key='c_g')
        self.load('sp', b_bc, dr['ln_b'][0, 0].partition_broadcast(128), writes=['b_bc'], key='c_b')
        self.load('sp', invc, dr['t_invc'][0].partition_broadcast(128).rearrange("p (g t) -> p g t", g=4), writes=['invc'], key='c_invc')
        self.load('sp', ident, dr['t_ident'], writes=['ident'], key='c_ident')
        self.cg_end()
        defA, defB = [], []
        for i in range(8):
            T0 = i * 512
            X = xh[i % 2]
            xk = ('xh', i % 2)
            if i == 0:
                P.op('pool', lambda e, X=X: e.memset(X[:, :, 0:16], 0.0), writes=[xk])
                self.load('sp', X[:, :, 16:528], xT_d[:, :, 0:512].rearrange("c p t -> p c t"), writes=[xk], key=('xh', 0))
            else:
                self.load('sp', X, xT_d[:, :, T0 - 16:T0 + 512].rearrange("c p t -> p c t"), writes=[xk], key=('xh', i % 2))
            DT = diffT[i % 2]
            dk = ('diffT', i % 2)
            for g in range(4):
                w = 2 ** (g + 1)
                xg = X[:, 2 * g:2 * g + 2, :]
                src, srck = xg, xk
                bufs = [(sA, 'sA'), (sB, 'sB')]
                sh = 1
                lo = 1
                for stp in range(g + 1):
                    dst, dstk = bufs[stp % 2]
                    P.op('dve', lambda e, dst=dst, src=src, lo=lo, sh=sh: e.tensor_tensor(
                        out=dst[:, :, lo:528], in0=src[:, :, lo:528], in1=src[:, :, lo - sh:528 - sh], op=ALU.add),
                        reads=[srck], writes=[dstk])
                    src, srck = dst, dstk
                    sh *= 2
                    lo += sh
                if i == 0:
                    P.op('dve', lambda e, src=src, g=g: e.tensor_tensor(
                        out=src[:, :, 16:528], in0=src[:, :, 16:528],
                        in1=invc[:, g:g + 1, :].to_broadcast([128, 2, 512]), op=ALU.mult), reads=[srck, 'invc'], writes=[srck])
                    P.op('dve', lambda e, src=src, xg=xg, DT=DT, g=g: e.tensor_tensor(
                        out=DT[:, 2 * g:2 * g + 2, :], in0=src[:, :, 16:528], in1=xg[:, :, 16:528], op=ALU.subtract),
                        reads=[srck, xk], writes=[dk])
                else:
                    P.op('dve', lambda e, src=src, xg=xg, DT=DT, g=g, w=w: e.scalar_tensor_tensor(
                        out=DT[:, 2 * g:2 * g + 2, :], in0=src[:, :, 16:528], scalar=1.0 / w, in1=xg[:, :, 16:528],
                        op0=ALU.mult, op1=ALU.subtract), reads=[srck, xk], writes=[dk])
            XS = xst[i % 2]
            xsk = ('xst', i % 2)
            for s in range(4):
                n = i * 4 + s
                q = n % NQ
                t0 = T0 + s * 128
                if defB:
                    defB.pop(0)()
                if defA:
                    defA.pop(0)()
                pa, pb = ps[2 * q], ps[2 * q + 1]
                pk = ('psA', q)
                self.load('sp', xtok[q], x_d[t0:t0 + 128, :], writes=[('xtok', q)], key=('xtok', q))
                for g in range(4):
                    pt = pa if g < 2 else pb
                    for kc in range(2):
                        self.mm(pt[:, (g % 2) * 256:(g % 2) * 256 + 256], DT[:, 2 * g + kc, s * 128:(s + 1) * 128],
                                poolw[:, g, kc, :], kc == 0, kc == 1, reads=[dk, 'poolw'], writes=[pk])
                P.op('dve', lambda e, q=q, pa=pa: e.tensor_tensor(out=hs[q][:, 0:512], in0=pa[:, :], in1=scale_bc[:, 0:512], op=ALU.mult),
                     reads=[pk, 'scale_bc'], writes=[('hs', q)])
                P.op('dve', lambda e, q=q, pb=pb: e.tensor_tensor(out=hs[q][:, 512:1024], in0=pb[:, :], in1=scale_bc[:, 512:1024], op=ALU.mult),
                     reads=[pk, 'scale_bc'], writes=[('hs', q)])
                P.op('dve', lambda e, q=q: e.scalar_tensor_tensor(out=z[q], in0=xtok[q], scalar=ALPHA, in1=hs[q], op0=ALU.mult, op1=ALU.add),
                     reads=[('xtok', q), ('hs', q)], writes=[('z', q)])
                tmp = dict(small[q]); tmp['xn'] = xn[q]
                self.ln1(z[q], ('z', q), tmp, 'A1ln%d' % q)

                def pieceA(q=q, n=n, t0=t0, s=s, i=i, XS=XS, xsk=xsk, tmp=tmp, T0=T0):
                    self.ln2(z[q], ('z', q), g_bc, b_bc, xo[q], ('xo', q), tmp, 'A1ln%d' % q)
                    self.load('act', x1_d[t0:t0 + 128, :], xo[q], reads=[('xo', q)], writes=[('x1_d', n)], key=('x1st', q))
                    self.copy('act', xb[q], xo[q], reads=[('xo', q)], writes=[('xb', q)])

                    def pieceB():
                        tb = ps[6 + n % 2][:, :].bitcast(BF16)
                        tk = ('pst', n % 2)
                        for c in range(8):
                            P.op('pe', lambda e, c=c: e.transpose(tb[:, c * 128:(c + 1) * 128], xb[q][:, c * 128:(c + 1) * 128], ident),
                                 reads=[('xb', q), 'ident'], writes=[tk])
                        self.copy('act', XS[:, :, s * 128:(s + 1) * 128], tb.rearrange("p (c t) -> p c t", c=8), reads=[tk], writes=[xsk])
                        if s == 3:
                            self.load('act', x1T_d[:, :, T0:T0 + 512].rearrange("c p t -> p c t"), XS, reads=[xsk], writes=[('x1T_d', i)], key=('xst', i % 2))
                    defB.append(pieceB)
                defA.append(pieceA)
        while defA or defB:
            if defB:
                defB.pop(0)()
            if defA:
                defA.pop(0)()
        P.barrier()

    def phase_A2(self, A, ps):
        P, nc, dr = self.P, self.nc, self.dram
        A.reset()
        x1_d, x1T_d, x2_d, x2T_d = dr['x1_d'], dr['x1T_d'], dr['x2_d'], dr['x2T_d']
        wgu, wdn = dr['ffn_w_gu'], dr['ffn_w_down']
        x1T = A.bf16(8 * 1024).rearrange("p (c t) -> p c t", c=8)
        wg = [A.bf16(8 * 512).rearrange("p (c f) -> p c f", c=8) for _ in range(2)]
        wu = [A.bf16(8 * 512).rearrange("p (c f) -> p c f", c=8) for _ in range(2)]
        actT = A.bf16(22 * 1024).rearrange("p (j t) -> p j t", j=22)
        wd = A.bf16(22 * 1024).rearrange("p (j d) -> p j d", j=22)
        g_bc, b_bc = A.f32(1024), A.f32(1024)
        ident = A.bf16(128)
        sg = [A.f32(512) for _ in range(2)]
        NQ = 3
        xtok = [A.f32(1024) for _ in range(NQ)]
        z = [A.f32(1024) for _ in range(NQ)]
        xn = z
        xo = [A.f32(1024) for _ in range(NQ)]
        xb = [A.bf16(1024) for _ in range(NQ)]
        xst = [A.bf16(8 * 128).rearrange("p (c t) -> p c t", c=8) for _ in range(NQ)]
        small = [dict(st=A.f32(12), mv=A.f32(2), rs=A.f32(1), nmr=A.f32(1)) for _ in range(NQ)]
        self.cg_begin('cgA2')
        self.load('sp', g_bc, dr['ln_g'][0, 1].partition_broadcast(128), writes=['g_bc'], key='c_g')
        self.load('sp', b_bc, dr['ln_b'][0, 1].partition_broadcast(128), writes=['b_bc'], key='c_b')
        self.load('sp', ident, dr['t_ident'], writes=['ident'], key='c_ident')
        self.cg_end()
        wdv = wdn.rearrange("(j p) d -> p j d", p=128)
        wguv = wgu.rearrange("(c p) f -> p c f", p=128)
        nblk = 6
        cnt = 0
        gu = 0
        defA, defB = [], []
        for st_ in range(4):
            T0 = st_ * 1024
            self.load('sp', x1T, x1T_d[:, :, T0:T0 + 1024].rearrange("c p t -> p c t"), writes=['x1T'], key='x1T')
            for fb in range(nblk):
                nch = 4 if fb < 5 else 2
                b = cnt % 2
                cnt += 1
                self.load('pool', wg[b][:, :, 0:nch * 128], wguv[:, :, fb * 512:fb * 512 + nch * 128], writes=[('wg', b)], key=('wg', b))
                self.load('pool', wu[b][:, :, 0:nch * 128], wguv[:, :, D_FF + fb * 512:D_FF + fb * 512 + nch * 128], writes=[('wu', b)], key=('wu', b))
                if st_ == 0 and fb == 1:
                    for j0 in range(0, 22, 6):
                        j1 = min(22, j0 + 6)
                        self.load('pool', wd[:, j0:j1, :], wdv[:, j0:j1, :], writes=[('wd', j0)], key=('wd', j0))
                for jj in range(nch):
                    j = fb * 4 + jj
                    for hf in range(2):
                        q = gu % 2
                        gu += 1
                        pg, pu = ps[2 * q], ps[2 * q + 1]
                        for kc in range(8):
                            self.mm(pg[:, :], wg[b][:, kc, jj * 128:(jj + 1) * 128], x1T[:, kc, hf * 512:(hf + 1) * 512],
                                    kc == 0, kc == 7, reads=[('wg', b), 'x1T'], writes=[('pg', q)])
                        for kc in range(8):
                            self.mm(pu[:, :], wu[b][:, kc, jj * 128:(jj + 1) * 128], x1T[:, kc, hf * 512:(hf + 1) * 512],
                                    kc == 0, kc == 7, reads=[('wu', b), 'x1T'], writes=[('pu', q)])
                        P.op('act', lambda e, q=q, pg=pg: e.activation(out=sg[q], in_=pg[:, :], func=AF.Silu),
                             reads=[('pg', q)], writes=[('sg', q)])
                        P.op('dve', lambda e, q=q, pu=pu, j=j, hf=hf: e.tensor_tensor(
                            out=actT[:, j, hf * 512:(hf + 1) * 512], in0=sg[q], in1=pu[:, :], op=ALU.mult),
                            reads=[('sg', q), ('pu', q)], writes=[('actT', j, hf)])
            for s in range(8):
                n = st_ * 8 + s
                qb = n % 2
                q = n % NQ
                t0 = T0 + s * 128
                if defB:
                    defB.pop(0)()
                if defA:
                    defA.pop(0)()
                self.load('sp', xtok[q], x1_d[t0:t0 + 128, :], reads=[('x1_d', n)], writes=[('xtok', q)], key=('xtok', q))
                pa, pb = ps[4 + 2 * qb], ps[5 + 2 * qb]
                pk = ('pdn', qb)
                for dh, pt in enumerate((pa, pb)):
                    for j in range(22):
                        self.mm(pt[:, :], actT[:, j, s * 128:(s + 1) * 128], wd[:, j, dh * 512:(dh + 1) * 512],
                                j == 0, j == 21, reads=[('actT', j, s // 4), ('wd', (j // 6) * 6)], writes=[pk])
                P.op('dve', lambda e, q=q, pa=pa: e.scalar_tensor_tensor(out=z[q][:, 0:512], in0=xtok[q][:, 0:512], scalar=ALPHA, in1=pa[:, :],
                                                                          op0=ALU.mult, op1=ALU.add), reads=[('xtok', q), pk], writes=[('z', q)])
                P.op('dve', lambda e, q=q, pb=pb: e.scalar_tensor_tensor(out=z[q][:, 512:1024], in0=xtok[q][:, 512:1024], scalar=ALPHA, in1=pb[:, :],
                                                                          op0=ALU.mult, op1=ALU.add), reads=[('xtok', q), pk], writes=[('z', q)])
                tmp = dict(small[q]); tmp['xn'] = xn[q]
                self.ln1(z[q], ('z', q), tmp, 'A2ln%d' % q)

                def pieceA(q=q, n=n, t0=t0, tmp=tmp):
                    self.ln2(z[q], ('z', q), g_bc, b_bc, xo[q], ('xo', q), tmp, 'A2ln%d' % q)
                    self.load('act', x2_d[t0:t0 + 128, :], xo[q], reads=[('xo', q)], writes=[('x2_d', n)], key=('x2st', q))
                    self.copy('act', xb[q], xo[q], reads=[('xo', q)], writes=[('xb', q)])

                    def pieceB():
                        tbk = ('pg', 0) if n % 2 == 0 else ('pu', 0)
                        tb = ps[n % 2][:, :].bitcast(BF16)
                        for c in range(8):
                            P.op('pe', lambda e, c=c: e.transpose(tb[:, c * 128:(c + 1) * 128], xb[q][:, c * 128:(c + 1) * 128], ident),
                                 reads=[('xb', q), 'ident'], writes=[tbk])
                        self.copy('act', xst[q], tb.rearrange("p (c t) -> p c t", c=8), reads=[tbk], writes=[('xst', q)])
                        self.load('act', x2T_d[:, :, t0:t0 + 128].rearrange("c p t -> p c t"), xst[q], reads=[('xst', q)], writes=[('x2T_d', n)], key=('xst', q))
                    defB.append(pieceB)
                defA.append(pieceA)
        while defA or defB:
            if defB:
                defB.pop(0)()
            if defA:
                defA.pop(0)()
        P.barrier()

    def phase_B(self, A, ps):
        P, nc, dr = self.P, self.nc, self.dram
        A.reset()
        x2_d, x2T_d, x3_d, qT_d = dr['x2_d'], dr['x2T_d'], dr['x3_d'], dr['qT_d']
        w_in, w_sw = dr['nsa_w_in'], dr['w_in_sw']
        bk = lambda i: ('bank', i)
        ksT = A.bf16(4 * S).rearrange("p (g t) -> p g t", g=4)
        kwT = A.bf16(4 * S).rearrange("p (g t) -> p g t", g=4)
        vs_aug = A.bf16(NT * 4 * 66).rearrange("p (n g d) -> p n g d", n=NT, g=4)
        vw_aug = A.bf16(NT * 4 * 66).rearrange("p (n g d) -> p n g d", n=NT, g=4)
        kcT = A.bf16(4 * 256).rearrange("p (g n) -> p g n", g=4)
        vc_aug = A.bf16(2 * 4 * 130).rearrange("p (c g d) -> p c g d", c=2, g=4)
        gates = A.f32(NT * 48).rearrange("p (n k) -> p n k", n=NT)
        ident = A.bf16(128)
        fold = A.bf16(64)
        self.cg_begin('cgB0')
        self.load('sp', ident, dr['t_ident'], writes=['ident'], key='c_ident')
        self.load('sp', fold, dr['t_fold'], writes=['fold'], key='c_fold')
        for g in range(4):
            self.load('sp', ksT[64:128, g, :], dr['t_E'], writes=[('ksE', g)], key=('c_E', g))
        self.cg_end()
        P.op('pool', lambda e: e.memset(vc_aug, 0.0), writes=['vc_aug0'])
        P.op('pool', lambda e: e.memset(vs_aug[:, :, :, 64:65], 1.0), writes=['vs_one'])
        P.op('pool', lambda e: e.memset(vw_aug[:, :, :, 64:65], 1.0), writes=['vw_one'])
        P.op('pool', lambda e: e.memset(kcT, 0.0), writes=['kcT0'])
        markP = A.off
        x2T = A.bf16(8 * S).rearrange("p (c t) -> p c t", c=8)
        for c in range(8):
            self.load('sp', x2T[:, c, :], x2T_d[c], writes=[('x2T', c)], key=('x2T', c))
        x2r = [('x2T', c) for c in range(8)]
        markX = A.off
        kvcr = kwT
        wblk = [A.bf16(8 * 128).rearrange("p (c f) -> p c f", c=8) for _ in range(2)]
        w1pad = A.bf16(2 * 32 * 128).rearrange("p (k l j) -> p k l j", k=2, l=32)
        pecol = A.bf16(64)
        penat = A.bf16(128)
        w2kk = A.bf16(128)
        w2v = A.bf16(64)
        csc = A.f32(256)
        biasb = A.f32(2)
        hid = [A.bf16(256) for _ in range(2)]
        Tb = [A.bf16(512) for _ in range(2)]
        P.op('pool', lambda e: e.memset(w1pad, 0.0), writes=['w1pad'])
        self.cg_begin('cgB1')
        self.load('pool', w1pad[0:64, 0, :, :], dr['nsa_w1_k'].rearrange("(l d) j -> d l j", d=64), reads=[], writes=['w1pad'], key='c_w1k')
        self.load('pool', w1pad[0:64, 1, :, :], dr['nsa_w1_v'].rearrange("(l d) j -> d l j", d=64), reads=['w1pad'], writes=['w1pad_v'], key='c_w1v')
        self.load('pool', penat[0:32, 0:64], dr['nsa_pe_k'], writes=['pen_k'], key='c_pek')
        self.load('pool', penat[0:32, 64:128], dr['nsa_pe_v'], writes=['pen_v'], key='c_pev')
        self.cg_end()
        tbp = ps[3][:, :].bitcast(BF16)
        for kv in range(2):
            P.op('pe', lambda e, kv=kv: e.transpose(tbp[0:64, kv * 32:(kv + 1) * 32], penat[0:32, kv * 64:(kv + 1) * 64], ident[0:32, 0:32]),
                 reads=['pen_k', 'pen_v', 'ident'], writes=[bk(3)])
        self.copy('dve', pecol[0:64, :], tbp[0:64, 0:64], reads=[bk(3)], writes=['pe_k', 'pe_v'])
        self.cg_begin('cgB1b')
        self.load('pool', w2kk[:, 0:64], dr['nsa_w2_k'], writes=['w2k_a'], key='c_w2k')
        self.load('pool', w2kk[:, 64:128], dr['w2k_sw'], writes=['w2k_b'], key='c_w2ks')
        self.load('pool', w2v, dr['nsa_w2_v'], writes=['w2v'], key='c_w2v')
        self.load('sp', csc, dr['t_csc'], writes=['csc'], key='c_csc')
        self.cg_end()
        nb = 0
        for g in range(4):
            b = nb % 2
            nb += 1
            self.load('pool', wblk[b][:, :, 0:64], w_in[:, 1024 + g * 64:1024 + (g + 1) * 64].rearrange("(c p) f -> p c f", p=128),
                      writes=[('wblk', b, 0)], key=('wblk', b, 0))
            self.load('pool', wblk[b][:, :, 64:128], w_in[:, 1280 + g * 64:1280 + (g + 1) * 64].rearrange("(c p) f -> p c f", p=128),
                      writes=[('wblk', b, 1)], key=('wblk', b, 1))
            for ti in range(8):
                for kv in range(2):
                    q = kv
                    dstT = kwT if kv == 0 else ksT
                    for c in range(8):
                        self.mm(ps[q][0:64, :], wblk[b][:, c, kv * 64:(kv + 1) * 64], x2T[:, c, ti * 512:(ti + 1) * 512], c == 0, c == 7,
                                reads=[('wblk', b, kv), ('x2T', c)], writes=[bk(q)])
                    self.copy('act' if kv == 0 else 'dve', dstT[0:64, g, ti * 512:(ti + 1) * 512], ps[q][0:64, :], reads=[bk(q)], writes=[('kvcr', kv, g, ti)])
        for kv in range(2):
            for l in range(32):
                self.mm(ps[2][:, kv:kv + 1], w1pad[0:64, kv, l, :], pecol[0:64, kv * 32 + l:kv * 32 + l + 1], l == 0, l == 31,
                        reads=['w1pad', 'w1pad_v', 'pe_k', 'pe_v'], writes=[bk(2)])
        self.copy('dve', biasb, ps[2][:, 0:2], reads=[bk(2)], writes=['biasb'])
        kvr = lambda g, kv: [('kvcr', kv, g, ti) for ti in range(8)]
        for g in range(4):
            for kv in range(2):
                q = kv
                pb_ = ps[4 + q]
                for l in range(32):
                    srcT = kwT if kv == 0 else ksT
                    self.mm(pb_[:, 0:255], w1pad[0:64, kv, l, :], srcT[0:64, g, l:l + 16 * 254 + 1:16], l == 0, l == 31,
                            reads=['w1pad', 'w1pad_v'] + kvr(g, kv), writes=[bk(4 + q)])
                P.op('act', lambda e, q=q, pb_=pb_, kv=kv: e.activation(out=hid[q][:, 0:255], in_=pb_[:, 0:255], func=AF.Silu, bias=biasb[:, kv:kv + 1]),
                     reads=[bk(4 + q), 'biasb'], writes=[('hid', q)])
                if g == 3 and getattr(self, 'debug', False):
                    dbt = A.f32(256)
                    self.copy('dve', dbt[:, 0:255], pb_[:, 0:255], reads=[bk(4 + q), ('hid', q)], writes=[('dbt', kv)])
                    self.dbg('dbg_pre%d' % kv, dbt, [128, 256], F32, [('dbt', kv)])
                if kv == 0:
                    self.mm(ps[6][:, 0:255], w2kk, hid[0][:, 0:255], True, True, reads=['w2k_a', 'w2k_b', ('hid', 0)], writes=[bk(6)])
                    P.op('dve', lambda e: e.tensor_tensor(out=Tb[0][:, 0:255], in0=ps[6][:, 0:255], in1=csc[:, 0:255], op=ALU.mult),
                         reads=[bk(6), 'csc'], writes=[('Tb', 0)])
                    self.mm(ps[7][0:64, 0:255], fold, Tb[0][:, 0:255], True, True, reads=['fold', ('Tb', 0)], writes=[bk(7)])
                    self.copy('act', kcT[0:64, g, 0:255], ps[7][0:64, 0:255], reads=[bk(7), 'kcT0'], writes=[('kcT', g)])
                else:
                    for c in range(2):
                        nn = 128 if c == 0 else 127
                        self.mm(ps[6][0:nn, 256 + c * 64:256 + (c + 1) * 64], hid[1][:, c * 128:c * 128 + nn], w2v, True, True,
                                reads=[('hid', 1), 'w2v'], writes=[bk(6)])
                        self.copy('dve', vc_aug[0:nn, c, g, 0:64], ps[6][0:nn, 256 + c * 64:256 + (c + 1) * 64],
                                  reads=[bk(6), 'vc_aug0'], writes=[('vc_aug', g)])
        self.dbg('dbg_vcr3', ksT[0:64, 3, :], [64, 4096], BF16, kvr(3, 1))
        self.dbg('dbg_kcr3', kwT[0:64, 3, :], [64, 4096], BF16, kvr(3, 0))
        self.dbg('dbg_w1v', w1pad[0:64, 1, :, :].rearrange("p l j -> p (l j)"), [64, 4096], BF16, ['w1pad', 'w1pad_v'])
        self.dbg('dbg_hid0', hid[0], [128, 256], BF16, [('hid', 0)])
        self.dbg('dbg_hid1', hid[1], [128, 256], BF16, [('hid', 1)])
        self.dbg('dbg_w1pa', w1pad[:, 0, 5, :], [128, 128], BF16, ['w1pad', 'w1pad_v'])
        self.dbg('dbg_w1pb', w1pad[:, 1, 5, :], [128, 128], BF16, ['w1pad', 'w1pad_v'])
        for l_ in (0, 17, 31):
            self.dbg('dbg_w1pb%d' % l_, w1pad[:, 1, l_, :], [128, 128], BF16, ['w1pad', 'w1pad_v'])
            self.dbg('dbg_w1pa%d' % l_, w1pad[:, 0, l_, :], [128, 128], BF16, ['w1pad', 'w1pad_v'])
        self.cg_begin('cgB2')
        for c in range(2):
            nn = 128 if c == 0 else 127
            P.op('pool', lambda e, c=c, nn=nn: e.memset(vc_aug[0:nn, c, :, 64:65], 1.0), reads=['vc_aug0'], writes=[('vc_one', c)])
            for g in range(4):
                self.load('sp', vc_aug[:, c, g, 65:129], dr['t_ovl'][c * 128:(c + 1) * 128, :], reads=['vc_aug0'], writes=[('vc_ovl', c, g)], key=('c_ovl', c, g))
        self.cg_end()
        vcr = [('vc_aug', g) for g in range(4)] + [('vc_one', c) for c in range(2)] + [('vc_ovl', c, g) for c in range(2) for g in range(4)]
        P.barrier()
        A.off = markX
        cst = [A.f32(512) for _ in range(2)]
        wv = A.bf16(8 * 512).rearrange("p (c f) -> p c f", c=8)
        wgt = A.bf16(8 * 48).rearrange("p (c f) -> p c f", c=8)
        wblk3 = [A.bf16(8 * 128).rearrange("p (c f) -> p c f", c=8) for _ in range(2)]
        Tb3 = [A.bf16(512) for _ in range(2)]
        qst = [A.bf16(512) for _ in range(2)]
        w_v = w_in.rearrange("(c p) f -> p c f", p=128)
        self.cg_begin('cgB3')
        self.load('pool', wv[:, :, 0:256], w_v[:, :, 1792:2048], reads=['w1pad', 'w1pad_v'], writes=['wv_a'], key='c_wv_a')
        self.load('pool', wv[:, :, 256:512], w_v[:, :, 2304:2560], reads=['w1pad', 'w1pad_v'], writes=['wv_b'], key='c_wv_b')
        self.load('pool', wgt, w_v[:, :, 2560:2608], reads=['w1pad', 'w1pad_v'], writes=['wgt'], key='c_wgt')
        self.cg_end()
        w_s = w_sw.rearrange("(c p) f -> p c f", p=128)
        nb = 0
        nq = 0
        deferred3 = []
        for blk in range(24):
            b = nb % 2
            nb += 1
            if blk < 16:
                natc, swc = blk * 64, blk * 64
            elif blk < 20:
                natc, swc = 1536 + (blk - 16) * 64, 1024 + (blk - 16) * 64
            else:
                natc, swc = 2048 + (blk - 20) * 64, 1280 + (blk - 20) * 64
            self.load('pool', wblk3[b][:, :, 0:64], w_v[:, :, natc:natc + 64], writes=[('wblk3', b, 0)], key=('wblk3', b, 0))
            self.load('pool', wblk3[b][:, :, 64:128], w_s[:, :, swc:swc + 64], writes=[('wblk3', b, 1)], key=('wblk3', b, 1))
            for ti in range(8):
                q = nq % 2
                nq += 1
                for c in range(8):
                    self.mm(ps[q][:, :], wblk3[b][:, c, :], x2T[:, c, ti * 512:(ti + 1) * 512], c == 0, c == 7,
                            reads=[('wblk3', b, 0), ('wblk3', b, 1), ('x2T', c)], writes=[bk(q)])
                self.load('sp', cst[q], dr['t_cs'][:, ti * 512:(ti + 1) * 512], writes=[('cst', q)], key=('cst', q))
                P.op('dve', lambda e, q=q, ti=ti: e.tensor_tensor(out=Tb3[q], in0=ps[q][:, :], in1=cst[q], op=ALU.mult),
                     reads=[bk(q), ('cst', q)], writes=[('Tb3', q)])
                def fold_part(q=q, blk=blk, ti=ti):
                    self.mm(ps[2 + q][0:64, :], fold, Tb3[q], True, True, reads=['fold', ('Tb3', q)], writes=[bk(2 + q)])
                    if blk < 16:
                        self.copy('act', qst[q][0:64, :], ps[2 + q][0:64, :], reads=[bk(2 + q)], writes=[('qst', q)])
                        self.load('act', qT_d[blk, :, ti * 512:(ti + 1) * 512], qst[q][0:64, :], reads=[('qst', q)], writes=[('qT_d', blk, ti)], key=('qst', q))
                    elif blk < 20:
                        self.copy('act', ksT[0:64, blk - 16, ti * 512:(ti + 1) * 512], ps[2 + q][0:64, :], reads=[bk(2 + q)], writes=[('ksT', blk - 16, ti)])
                    else:
                        self.copy('act', kwT[0:64, blk - 20, ti * 512:(ti + 1) * 512], ps[2 + q][0:64, :], reads=[bk(2 + q)], writes=[('kwT', blk - 20, ti)])
                deferred3.append(fold_part)
                while len(deferred3) > 1:
                    deferred3.pop(0)()
        while deferred3:
            deferred3.pop(0)()
        for n in range(NT):
            q = n % 2
            for c in range(8):
                self.mm(ps[4 + q][:, :], x2T[:, c, n * 128:(n + 1) * 128], wv[:, c, :], c == 0, c == 7,
                        reads=[('x2T', c), 'wv_a', 'wv_b'], writes=[bk(4 + q)])
            for c in range(8):
                self.mm(ps[6 + q][:, 0:48], x2T[:, c, n * 128:(n + 1) * 128], wgt[:, c, :], c == 0, c == 7,
                        reads=[('x2T', c), 'wgt'], writes=[bk(6 + q)])
            self.copy('dve', vs_aug[:, n, :, 0:64], ps[4 + q][:, 0:256].rearrange("p (g d) -> p g d", g=4), reads=[bk(4 + q)], writes=[('vs', n)])
            self.copy('dve', vw_aug[:, n, :, 0:64], ps[4 + q][:, 256:512].rearrange("p (g d) -> p g d", g=4), reads=[bk(4 + q)], writes=[('vw', n)])
            P.op('act', lambda e, n=n, q=q: e.activation(out=gates[:, n, :], in_=ps[6 + q][:, 0:48], func=AF.Sigmoid),
                 reads=[bk(6 + q)], writes=[('gates', n)])
        self.dbg('dbg_bias', biasb, [128, 2], F32, ['biasb'])
        self.dbg('dbg_kcT', kcT.rearrange("p g n -> p (g n)"), [128, 1024], BF16, [('kcT', g) for g in range(4)])
        self.dbg('dbg_vc', vc_aug.rearrange("p c g d -> p (c g d)"), [128, 1040], BF16, vcr)
        self.dbg('dbg_ksT', ksT[:, 0, 0:512], [128, 512], BF16, [('ksT', 0, 0), ('ksE', 0)])
        self.dbg('dbg_kwT', kwT[:, 1, 512:1024], [128, 512], BF16, [('kwT', 1, 1)])
        self.dbg('dbg_vs', vs_aug[:, 0:2, :, :].rearrange("p n g d -> p (n g d)"), [128, 528], BF16, [('vs', 0), ('vs', 1), 'vs_one'])
        self.dbg('dbg_gates', gates[:, 0, :], [128, 48], F32, [('gates', 0)])
        P.barrier()
        A.off = markP
        wout = A.bf16(8 * 1024).rearrange("p (c d) -> p c d", c=8)
        g_bc, b_bc = A.f32(1024), A.f32(1024)
        tB = A.bf16(S)
        trilo = A.bf16(512).rearrange("p (h t) -> p h t", h=4)
        trihi = A.bf16(512).rearrange("p (h t) -> p h t", h=4)
        Qa = [A.bf16(16 * 128).rearrange("p (h t) -> p h t", h=16) for _ in range(2)]
        PT = [A.bf16(512) for _ in range(4)]
        bonus = [A.f32(64) for _ in range(2)]
        valid = [A.f32(64) for _ in range(2)]
        negpad = [A.bf16(128) for _ in range(2)]
        ocomb = [A.bf16(1024) for _ in range(2)]
        oT = [A.bf16(1024).rearrange("p (c t) -> p c t", c=8) for _ in range(2)]
        oc = [A.f32(256).rearrange("p (h d) -> p h d", h=4) for _ in range(2)]
        otmp = [A.f32(256).rearrange("p (h d) -> p h d", h=4) for _ in range(2)]
        selb = [dict(rc=A.f32(4), imp=A.f32(64), score=A.f32(64), sc2=A.f32(64), mxa=A.f32(8), mxb=A.f32(8), sel=A.f32(64),
                     rr=A.f32(12)) for _ in range(2)]
        xtok = [A.f32(1024) for _ in range(2)]
        z = [A.f32(1024)] * 2
        xn = [A.f32(1024)] * 2
        xo = [A.f32(1024) for _ in range(2)]
        small = [dict(st=A.f32(12), mv=A.f32(2), rs=A.f32(1), nmr=A.f32(1))] * 2
        self.cg_begin('cgB4')
        self.load('sp', tB, dr['t_B'], writes=['tB'], key='c_tB')
        self.load('sp', trilo, dr['t_trilo4'].rearrange("p (h t) -> p h t", h=4), writes=['trilo'], key='c_trilo')
        self.load('sp', trihi, dr['t_trihi4'].rearrange("p (h t) -> p h t", h=4), writes=['trihi'], key='c_trihi')
        self.load('pool', wout, dr['nsa_w_out'].rearrange("(c p) d -> p c d", p=128), writes=['wout'], key='c_wout')
        self.load('sp', g_bc, dr['ln_g'][1, 0].partition_broadcast(128), writes=['g_bc'], key='c_g')
        self.load('sp', b_bc, dr['ln_b'][1, 0].partition_broadcast(128), writes=['b_bc'], key='c_b')
        self.cg_end()
        for q in range(2):
            P.op('pool', lambda e, q=q: e.memset(negpad[q], 0.0), writes=[('negpad', q)])
        zl = A.bf16(128)
        zr = A.bf16(260)
        P.op('pool', lambda e: e.memset(zl, 0.0), writes=['zl'])
        P.op('pool', lambda e: e.memset(zr, 0.0), writes=['zr'])

        def zero_acc(bank, w_):
            self.mm(ps[bank][:, 0:w_], zl, zr[:, 0:w_], True, False, reads=['zl', 'zr'], writes=[bk(bank)])
        cnt = dict(ns=0, npt=0, ng=0)
        SB = (0, 1, 7)
        NPT = len(PT)
        LAG = 2
        pending = []

        delayed = []

        def push(fn):
            pending.append(fn)
            while len(pending) > LAG:
                pending.pop(0)()
            for d_ in list(delayed):
                d_[0] -= 1
                if d_[0] <= 0:
                    delayed.remove(d_)
                    d_[1]()

        def score(lhsT, rhs, mask, lreads, qi, g):
            bq = SB[cnt['ns'] % 3]
            cnt['ns'] += 1
            S_ = ps[bq][:, :].rearrange("p (h t) -> p h t", h=4)
            self.mm(S_, lhsT, rhs, True, mask is None, reads=lreads + [('Qa', qi), ('Qneg', qi, g)], writes=[bk(bq)])
            if mask is not None:
                self.mm(S_, ident, mask[0], False, True, reads=['ident'] + mask[1], writes=[bk(bq)])
            pq = cnt['npt'] % NPT
            cnt['npt'] += 1
            P.op('act', lambda e, bq=bq, pq=pq: e.activation(out=PT[pq], in_=ps[bq][:, :], func=AF.Exp, scale=ATTN_SCALE),
                 reads=[bk(bq)], writes=[('PT', pq)])
            return pq

        def norm_branch(bank, br, i, g, gq, qi, sb_):
            sk = lambda nm: ('sel', nm, gq)
            rr = sb_['rr'].rearrange("p (b h) -> p b h", b=3)
            acc = ps[bank][:, 0:260].rearrange("p (h d) -> p h d", h=4)
            P.op('dve', lambda e: e.tensor_scalar(out=rr[:, br, :], in0=acc[:, :, 64], scalar1=1e-30, scalar2=None, op0=ALU.max),
                 reads=[bk(bank)], writes=[sk('rr%d' % br)])
            P.op('dve', lambda e: e.reciprocal(out=rr[:, br, :], in_=rr[:, br, :]), reads=[sk('rr%d' % br)], writes=[sk('rr%d' % br)])
            P.op('dve', lambda e: e.tensor_tensor(out=rr[:, br, :], in0=rr[:, br, :],
                                                  in1=gates[:, i, :].rearrange("p (h b) -> p h b", b=3)[:, 4 * g:4 * g + 4, br], op=ALU.mult),
                 reads=[sk('rr%d' % br), ('gates', i)], writes=[sk('rr%d' % br)])
            dst = oc[gq] if br == 0 else otmp[gq]
            P.op('dve', lambda e: e.tensor_tensor(out=dst, in0=acc[:, :, 0:64], in1=rr[:, br, :].unsqueeze(2).to_broadcast([128, 4, 64]), op=ALU.mult),
                 reads=[bk(bank), sk('rr%d' % br)], writes=[sk('oc') if br == 0 else sk('otmp')])
            if br == 2:
                P.op('dve', lambda e: e.tensor_tensor(out=oc[gq], in0=oc[gq], in1=otmp[gq], op=ALU.add),
                     reads=[sk('oc'), sk('otmp')], writes=[sk('oc')])
            if br == 1:
                P.op('dve', lambda e: e.tensor_tensor(out=ocomb[qi][:, g * 256:(g + 1) * 256].rearrange("p (h d) -> p h d", h=4),
                                                      in0=oc[gq], in1=otmp[gq], op=ALU.add),
                     reads=[sk('oc'), sk('otmp')], writes=[('ocomb', qi, g)])

        def selection(i, g, gq, qi, sb_):
            sk = lambda nm: ('sel', nm, gq)
            accC = ps[2][:, 0:260].rearrange("p (h d) -> p h d", h=4)
            accI = ps[3][:, 0:256].rearrange("p (h d) -> p h d", h=4)
            P.op('dve', lambda e: e.tensor_scalar(out=sb_['rc'], in0=accC[:, :, 64], scalar1=1e-30, scalar2=None, op0=ALU.max),
                 reads=[bk(2)], writes=[sk('rc')])
            P.op('dve', lambda e: e.reciprocal(out=sb_['rc'], in_=sb_['rc']), reads=[sk('rc')], writes=[sk('rc')])
            P.op('dve', lambda e: e.tensor_scalar(out=sb_['imp'], in0=accI[:, 0, :], scalar1=sb_['rc'][:, 0:1], scalar2=None, op0=ALU.mult),
                 reads=[bk(3), sk('rc')], writes=[sk('imp')])
            for h in range(1, 4):
                P.op('dve', lambda e, h=h: e.scalar_tensor_tensor(out=sb_['imp'], in0=accI[:, h, :], scalar=sb_['rc'][:, h:h + 1],
                                                                  in1=sb_['imp'], op0=ALU.mult, op1=ALU.add),
                     reads=[bk(3), sk('rc'), sk('imp')], writes=[sk('imp')])
            P.op('dve', lambda e: e.tensor_tensor(out=sb_['score'], in0=sb_['imp'], in1=bonus[qi], op=ALU.add),
                 reads=[sk('imp'), ('bonus', qi)], writes=[sk('score')])
            P.op('dve', lambda e: e.max(out=sb_['mxa'], in_=sb_['score']), reads=[sk('score')], writes=[sk('mxa')])
            P.op('dve', lambda e: e.match_replace(out=sb_['sc2'], in_to_replace=sb_['mxa'], in_values=sb_['score'], imm_value=-1e30),
                 reads=[sk('score'), sk('mxa')], writes=[sk('sc2')])
            P.op('dve', lambda e: e.max(out=sb_['mxb'], in_=sb_['sc2']), reads=[sk('sc2')], writes=[sk('mxb')])
            P.op('dve', lambda e: e.tensor_scalar(out=sb_['sel'], in0=sb_['score'], scalar1=sb_['mxb'][:, 7:8], scalar2=None, op0=ALU.is_ge),
                 reads=[sk('score'), sk('mxb')], writes=[sk('sel')])
            P.op('dve', lambda e: e.tensor_tensor(out=sb_['sel'], in0=sb_['sel'], in1=valid[qi], op=ALU.mult),
                 reads=[sk('sel'), ('valid', qi)], writes=[sk('sel')])
            P.op('dve', lambda e: e.tensor_scalar(out=negpad[gq][:, 64:128], in0=sb_['sel'], scalar1=-NEG, scalar2=NEG, op0=ALU.mult, op1=ALU.add),
                 reads=[sk('sel')], writes=[('negpad', gq)])
            norm_branch(2, 0, i, g, gq, qi, sb_)

        def tile_tail(i, qi):
            T0 = i * 128
            tb6 = ps[6][:, :].bitcast(BF16)
            for c in range(8):
                P.op('pe', lambda e, c=c: e.transpose(tb6[:, c * 128:(c + 1) * 128], ocomb[qi][:, c * 128:(c + 1) * 128], ident),
                     reads=[('ocomb', qi, c // 2), 'ident'], writes=[bk(6)])
            self.copy('act', oT[qi], tb6.rearrange("p (c t) -> p c t", c=8), reads=[bk(6)], writes=[('oT', qi)])
            for dh in range(2):
                for c in range(8):
                    self.mm(ps[6][:, :], oT[qi][:, c, :], wout[:, c, dh * 512:(dh + 1) * 512], c == 0, c == 7,
                            reads=[('oT', qi), 'wout'], writes=[bk(6)])
                P.op('dve', lambda e, dh=dh: e.scalar_tensor_tensor(out=z[qi][:, dh * 512:(dh + 1) * 512], in0=xtok[qi][:, dh * 512:(dh + 1) * 512],
                                                                    scalar=ALPHA, in1=ps[6][:, :], op0=ALU.mult, op1=ALU.add),
                     reads=[('xtok', qi), bk(6)], writes=[('z', 0)])
            tmp = dict(small[qi]); tmp['xn'] = xn[qi]
            self.ln1(z[qi], ('z', 0), tmp, 'B4ln')

            def tail_b():
                self.ln2a(z[qi], ('z', 0), tmp, 'B4ln')

                def tail_c():
                    self.ln2b(g_bc, b_bc, xo[qi], ('xo', qi), tmp, 'B4ln', mul_eng='dve')
                    self.load('sp', x3_d[T0:T0 + 128, :], xo[qi], reads=[('xo', qi)], writes=[('x3_d', i)], key=('x3st', qi))
                delayed.append([2, tail_c])
            delayed.append([2, tail_b])

        def do_group(i, g, qi, Q):
            T0 = i * 128
            gq = cnt['ng'] % 2
            cnt['ng'] += 1
            sb_ = selb[gq]
            Qg = Q[0:64, 4 * g:4 * g + 4, :]
            Qg_full = Q[:, 4 * g:4 * g + 4, :]
            nch = 1 if i < 16 else 2

            def cmp_pv(c, pq, last):
                if c == 0:
                    zero_acc(2, 260)
                    zero_acc(3, 256)
                for h in range(4):
                    self.mm(ps[2][:, h * 65:(h + 1) * 65], PT[pq][:, h * 128:(h + 1) * 128], vc_aug[:, c, g, 0:65], False, last,
                            reads=[('PT', pq)] + vcr, writes=[bk(2)])
                    self.mm(ps[3][:, h * 64:(h + 1) * 64], PT[pq][:, h * 128:(h + 1) * 128], vc_aug[:, c, g, 65:129], False, last,
                            reads=[('PT', pq)] + vcr, writes=[bk(3)])
                if last:
                    selection(i, g, gq, qi, sb_)
            for c in range(nch):
                a0 = T0 - 2048 * c
                pq = score(kcT[0:64, g, c * 128:(c + 1) * 128], Qg,
                           (tB[:, a0:a0 + 128].unsqueeze(1).to_broadcast([128, 4, 128]), ['tB']), [('kcT', g), 'kcT0'], qi, g)
                f_ = lambda c=c, pq=pq: cmp_pv(c, pq, c == nch - 1)
                f_._cmp = True
                push(f_)
            wch = [m for m in range(5) if T0 - 512 + 128 * m >= 0]

            def win_pv(wi, kc0, pq, last):
                if wi == 0:
                    zero_acc(4, 260)
                for h in range(4):
                    self.mm(ps[4][:, h * 65:(h + 1) * 65], PT[pq][:, h * 128:(h + 1) * 128], vw_aug[:, kc0 // 128, g, 0:65],
                            False, last, reads=[('PT', pq), ('vw', kc0 // 128), 'vw_one'], writes=[bk(4)])
                if last:
                    norm_branch(4, 2, i, g, gq, qi, sb_)
            def emit_negmask():
                while any(getattr(f, '_cmp', False) for f in pending):
                    pending.pop(0)()
                tb = ps[6][:, :].bitcast(BF16)
                P.op('pe', lambda e: e.transpose(tb[:, 0:128], negpad[gq], ident), reads=[('negpad', gq), 'ident'], writes=[bk(6)])
                self.copy('act', Q[64:128, 4 * g:4 * g + 4, :], tb[64:128, 0:128].unsqueeze(1).to_broadcast([64, 4, 128]),
                          reads=[bk(6)], writes=[('Qneg', qi, g)])
            for wi, m in enumerate(wch):
                kc0 = T0 - 512 + 128 * m
                mask = None
                if m == 0:
                    mask = (trihi, ['trihi'])
                elif m == 4:
                    mask = (trilo, ['trilo'])
                pq = score(kwT[0:64, g, kc0:kc0 + 128], Qg, mask, [('kwT', g, kc0 // 512)], qi, g)
                push(lambda wi=wi, kc0=kc0, pq=pq: win_pv(wi, kc0, pq, wi == len(wch) - 1))
            emit_negmask()

            def sel_pv(c, pq, last):
                if c == 0:
                    zero_acc(5, 260)
                for h in range(4):
                    self.mm(ps[5][:, h * 65:(h + 1) * 65], PT[pq][:, h * 128:(h + 1) * 128], vs_aug[:, c, g, 0:65],
                            False, last, reads=[('PT', pq), ('vs', c), 'vs_one'], writes=[bk(5)])
                if last:
                    norm_branch(5, 1, i, g, gq, qi, sb_)
                    if g == 3:
                        delayed.append([3, lambda: tile_tail(i, qi)])
            for c in range(i + 1):
                mask = (trilo, ['trilo']) if c == i else None
                pq = score(ksT[:, g, c * 128:(c + 1) * 128], Qg_full, mask, [('ksT', g, c // 4), ('ksE', g)], qi, g)
                push(lambda c=c, pq=pq: sel_pv(c, pq, c == i))

        def do_tile(i):
            T0 = i * 128
            qi = i % 2
            Q = Qa[qi]
            self.load('sp', Q[0:64, :, :], qT_d[:, :, T0:T0 + 128].rearrange("h d t -> d h t"), writes=[('Qa', qi)], key=('Qa', qi))
            self.load('sp', bonus[qi], dr['t_bonus'][T0:T0 + 128, :], writes=[('bonus', qi)], key=('bonus', qi))
            self.load('sp', valid[qi], dr['t_valid'][T0:T0 + 128, :], writes=[('valid', qi)], key=('valid', qi))
            self.load('sp', xtok[qi], x2_d[T0:T0 + 128, :], writes=[('xtok', qi)], key=('xtok', qi))
            for g in range(4):
                do_group(i, g, qi, Q)
        for i in range(NT):
            do_tile(i)
        while pending:
            pending.pop(0)()
        for d_ in delayed:
            d_[1]()
        P.barrier()

    def phase_C(self, A, ps):
        P, nc, dr = self.P, self.nc, self.dram
        A.reset()
        x3_d, out_d, Xg_d, Yg_d = dr['x3_d'], dr['out'], dr['Xg_d'], dr['Yg_d']
        wgu_d, wdn_d = dr['moe_w_gu'], dr['moe_w_down']
        bk = lambda i: ('bank', i)
        NSLOT = NEXP * CAP
        slots_i = A.i32(NT * 2)
        icur = A.i32(1)
        wts = A.f32(NT * 2).rearrange("p (n k) -> p n k", k=2)
        carry = A.f32(8)
        g_bc, b_bc = A.f32(1024), A.f32(1024)
        ident32 = A.f32(128)
        identb = A.bf16(128)
        triU = A.bf16(128)
        onesb = A.bf16(128)
        ebase = A.f32(8)
        router = A.f32(64).rearrange("p (c e) -> p c e", c=8)
        self.cg_begin('cgC')
        self.load('sp', g_bc, dr['ln_g'][1, 1].partition_broadcast(128), writes=['g_bc'], key='c_g')
        self.load('sp', b_bc, dr['ln_b'][1, 1].partition_broadcast(128), writes=['b_bc'], key='c_b')
        self.load('sp', ident32, dr['t_ident32'], writes=['ident32'], key='c_i32')
        self.load('sp', identb, dr['t_ident'], writes=['identb'], key='c_ident')
        self.load('sp', triU, dr['t_triU'], writes=['triU'], key='c_triU')
        self.load('sp', onesb, dr['t_ones'], writes=['onesb'], key='c_ones')
        self.load('sp', ebase, dr['t_ebase'][0].partition_broadcast(128), writes=['ebase'], key='c_ebase')
        self.load('sp', router, dr['moe_router'].rearrange("(c p) e -> p c e", p=128), writes=['router'], key='c_router')
        self.cg_end()
        P.op('dve', lambda e: e.memset(carry, 0.0), writes=['carry'])
        mark2 = A.off
        xt = [A.f32(1024) for _ in range(3)]
        xb_all = A.bf16(NT * 1024).rearrange("p (n d) -> p n d", n=NT)
        xT32 = [A.f32(1024).rearrange("p (c t) -> p c t", c=8) for _ in range(3)]
        sm = [dict(lg=A.f32(8), mx=A.f32(8), oh1=A.f32(8), oh2=A.f32(8), mk=A.bf16(8), pos=A.f32(8), sv=A.f32(8),
                   tmp=A.f32(8), s12=A.f32(2), dd=A.f32(2)) for _ in range(3)]
        zt = xT32[0].rearrange("p c t -> p (c t)").bitcast(BF16).rearrange("p (a d) -> p a d", a=2)
        P.op('pool', lambda e: e.memset(zt, 0.0), writes=[('xT32a', 0), ('xT32b', 0)])
        self.load('sp', Yg_d[NSLOT:NSLOT + 1, :], zt[0:1, :, :].rearrange("p a d -> p (a d)"), reads=[('xT32a', 0), ('xT32b', 0)],
                  writes=['Yg_zero'], key='ygz')
        for r0 in range(0, NSLOT, 256):
            self.load('sp', Xg_d[r0:r0 + 256, :].rearrange("(a p) d -> p a d", p=128), zt, reads=[('xT32a', 0), ('xT32b', 0)],
                      writes=['Xg_zero'], key='xgz')
        def stageA(n):
            q, qb, t0 = n % 3, n % 2, n * 128
            k_ = lambda name: (name, q)
            self.load('sp', xt[q], x3_d[t0:t0 + 128, :], writes=[k_('xt')], key=('xt', q))
            self.copy('act', xb_all[:, n, :], xt[q], reads=[k_('xt')], writes=[('xb', n)])
            pa, pb = ps[2 * qb], ps[2 * qb + 1]
            for c in range(8):
                pt = pa if c < 4 else pb
                self.mm(pt[:, (c % 4) * 128:(c % 4) * 128 + 128], xt[q][:, c * 128:(c + 1) * 128], ident32, True, True,
                        reads=[k_('xt'), 'ident32'], writes=[bk(2 * qb + (0 if c < 4 else 1))])
            self.copy('act', xT32[q][:, 0:4, :], pa[:, :].rearrange("p (c t) -> p c t", c=4), reads=[bk(2 * qb)], writes=[k_('xT32a')])
            self.copy('dve', xT32[q][:, 4:8, :], pb[:, :].rearrange("p (c t) -> p c t", c=4), reads=[bk(2 * qb + 1)], writes=[k_('xT32b')])
            pl = ps[4 + qb]
            for c in range(8):
                self.mm(pl[:, 0:8], xT32[q][:, c, :], router[:, c, :], c == 0, c == 7,
                        reads=[k_('xT32a'), k_('xT32b'), 'router'], writes=[bk(4 + qb)])

        def stageB(n):
            q, qb = n % 3, n % 2
            m = sm[q]
            k_ = lambda name: (name, q)
            pl = ps[4 + qb]
            self.copy('dve', m['lg'], pl[:, 0:8], reads=[bk(4 + qb)], writes=[k_('lg')])
            P.op('dve', lambda e: e.max(out=m['mx'], in_=m['lg']), reads=[k_('lg')], writes=[k_('mx')])
            P.op('dve', lambda e: e.tensor_scalar(out=m['oh1'], in0=m['lg'], scalar1=m['mx'][:, 0:1], scalar2=None, op0=ALU.is_equal),
                 reads=[k_('lg'), k_('mx')], writes=[k_('oh1')])
            P.op('dve', lambda e: e.tensor_scalar(out=m['oh2'], in0=m['lg'], scalar1=m['mx'][:, 1:2], scalar2=None, op0=ALU.is_equal),
                 reads=[k_('lg'), k_('mx')], writes=[k_('oh2')])
            P.op('dve', lambda e: e.tensor_tensor(out=m['dd'][:, 0:1], in0=m['mx'][:, 0:1], in1=m['mx'][:, 1:2], op=ALU.subtract),
                 reads=[k_('mx')], writes=[k_('dd')])
            P.op('act', lambda e: e.activation(out=wts[:, n, 0:1], in_=m['dd'][:, 0:1], func=AF.Sigmoid),
                 reads=[k_('dd')], writes=[('wts', n)])
            P.op('act', lambda e: e.activation(out=wts[:, n, 1:2], in_=m['dd'][:, 0:1], func=AF.Sigmoid, scale=-1.0),
                 reads=[k_('dd')], writes=[('wts', n)])
            P.op('dve', lambda e: e.tensor_tensor(out=m['mk'], in0=m['oh1'], in1=m['oh2'], op=ALU.add),
                 reads=[k_('oh1'), k_('oh2')], writes=[k_('mk')])
            pp = ps[6 + qb]
            self.mm(pp[:, 0:8], triU, m['mk'], True, True, reads=['triU', k_('mk')], writes=[bk(6 + qb)])
            self.mm(pp[:, 8:16], onesb, m['mk'], True, True, reads=['onesb', k_('mk')], writes=[bk(6 + qb)])

        def stageC(n):
            q, qb = n % 3, n % 2
            m = sm[q]
            k_ = lambda name: (name, q)
            pp = ps[6 + qb]
            P.op('dve', lambda e: e.tensor_tensor(out=m['pos'], in0=pp[:, 0:8], in1=carry, op=ALU.add),
                 reads=[bk(6 + qb), 'carry'], writes=[k_('pos')])
            P.op('dve', lambda e: e.tensor_tensor(out=carry, in0=pp[:, 8:16], in1=carry, op=ALU.add),
                 reads=[bk(6 + qb), 'carry'], writes=['carry'])
            P.op('dve', lambda e: e.tensor_scalar(out=m['tmp'], in0=m['pos'], scalar1=float(CAP), scalar2=1.0e6, op0=ALU.is_ge, op1=ALU.mult),
                 reads=[k_('pos')], writes=[k_('tmp')])
            P.op('dve', lambda e: e.tensor_tensor(out=m['sv'], in0=m['pos'], in1=ebase, op=ALU.add),
                 reads=[k_('pos'), 'ebase'], writes=[k_('sv')])
            P.op('dve', lambda e: e.tensor_tensor(out=m['sv'], in0=m['sv'], in1=m['tmp'], op=ALU.add),
                 reads=[k_('sv'), k_('tmp')], writes=[k_('sv')])
            P.op('dve', lambda e: e.tensor_scalar(out=m['sv'], in0=m['sv'], scalar1=float(NSLOT), scalar2=None, op0=ALU.min),
                 reads=[k_('sv')], writes=[k_('sv')])
            for w_, oh in enumerate(('oh1', 'oh2')):
                P.op('dve', lambda e, oh=oh: e.tensor_tensor(out=m['tmp'], in0=m[oh], in1=m['sv'], op=ALU.mult),
                     reads=[k_(oh), k_('sv')], writes=[k_('tmp')])
                P.op('dve', lambda e, w_=w_: e.reduce_sum(out=m['s12'][:, w_:w_ + 1], in_=m['tmp'], axis=mybir.AxisListType.X),
                     reads=[k_('tmp')], writes=[k_('s12')])
            P.op('dve', lambda e: e.tensor_copy(out=slots_i[:, 2 * n:2 * n + 2], in_=m['s12'][:, 0:2]), reads=[k_('s12')], writes=[('slots', n)])

        for n in range(NT + 2):
            if n < NT:
                stageA(n)
            if 0 <= n - 1 < NT:
                stageB(n - 1)
            if 0 <= n - 2 < NT:
                stageC(n - 2)

        def scat_fn(g, sem):
            base = self.loop_cnt
            with g.Fori(0, 2 * NT) as i:
                g.tensor_copy(out=icur, in_=slots_i[:, bass.ts(i, 1)]).then_inc(self.loop_sem, 1)
                g.wait_ge(self.loop_sem, base + i + 1)
                g.indirect_dma_start(out=Xg_d, out_offset=bass.IndirectOffsetOnAxis(ap=icur, axis=0),
                                     in_=xb_all[:, i // 2, :], in_offset=None,
                                     bounds_check=NSLOT - 1, oob_is_err=False).then_inc(sem, 16)
                g.wait_ge(sem, 16 * i + 16)
            self.loop_cnt += 2 * NT
        P.dma('pool', scat_fn, reads=[('xb', n) for n in range(NT)] + [('slots', n) for n in range(NT)] + ['Xg_zero'],
              writes=['Xg_d', 'icur', 'Xg_zero'], key='scatloop', count=2 * NT, raw=True)
        P.barrier()
        A.off = mark2
        TT = [(0, 512), (512, 512), (1024, CAP - 1024)]
        XgT = [A.bf16(8 * CAP).rearrange("p (c t) -> p c t", c=8) for _ in range(2)]
        wd = [A.bf16(14 * 1024).rearrange("p (j d) -> p j d", j=14) for _ in range(2)]
        wg = [A.bf16(8 * 256).rearrange("p (c f) -> p c f", c=8) for _ in range(3)]
        wu = [A.bf16(8 * 256).rearrange("p (c f) -> p c f", c=8) for _ in range(3)]
        actT = A.bf16(14 * CAP).rearrange("p (j t) -> p j t", j=14)
        xg = [A.bf16(1024) for _ in range(2)]
        ysb = [A.bf16(1024) for _ in range(2)]
        sg = [A.f32(512) for _ in range(2)]
        cnts = dict(blk=0, gu=0, dn=0, xg=0)
        Ygv = Yg_d[0:NSLOT, :].rearrange("s (h d) -> s h d", h=2)

        def load_xgT(e_):
            X = XgT[e_ % 2]
            for s_ in range(CAP // 128):
                q = cnts['xg'] % 2
                cnts['xg'] += 1
                self.load('sp', xg[q], Xg_d[e_ * CAP + s_ * 128:e_ * CAP + (s_ + 1) * 128, :], writes=[('xg', q)], key=('xg', q))
                tb = ps[4 + q][:, :].bitcast(BF16)
                for c in range(8):
                    P.op('pe', lambda e, c=c, q=q, tb=tb: e.transpose(tb[:, c * 128:(c + 1) * 128], xg[q][:, c * 128:(c + 1) * 128], identb),
                         reads=[('xg', q), 'identb'], writes=[bk(4 + q)])
                self.copy(self.evac_eng(), X[:, :, s_ * 128:(s_ + 1) * 128], tb.rearrange("p (c t) -> p c t", c=8),
                          reads=[bk(4 + q)], writes=[('XgT', e_ % 2, s_)])

        load_xgT(0)
        for u in range(2 * NEXP):
            e_, half = u // 2, u % 2
            X = XgT[e_ % 2]
            W = wd[u % 2]
            f0 = half * (DFE // 2)
            self.load('pool', W, wdn_d[e_, f0:f0 + DFE // 2, :].rearrange("(j p) d -> p j d", p=128), writes=[('wd', u % 2)], key=('wd', u % 2))
            wv = wgu_d[e_].rearrange("(c p) f -> p c f", p=128)
            for fb in range(7):
                b = cnts['blk'] % 3
                cnts['blk'] += 1
                self.load('pool', wg[b], wv[:, :, f0 + fb * 256:f0 + fb * 256 + 256], writes=[('wg', b)], key=('wg', b))
                self.load('pool', wu[b], wv[:, :, DFE + f0 + fb * 256:DFE + f0 + fb * 256 + 256], writes=[('wu', b)], key=('wu', b))
                for jj in range(2):
                    j = fb * 2 + jj
                    for ti, (ta, tn) in enumerate(TT):
                        q = cnts['gu'] % 2
                        cnts['gu'] += 1
                        pg, pu = ps[2 * q], ps[2 * q + 1]
                        xr = [('XgT', e_ % 2, s_) for s_ in range(ta // 128, (ta + tn) // 128)]
                        for kc in range(8):
                            self.mm(pg[:, 0:tn], wg[b][:, kc, jj * 128:(jj + 1) * 128], X[:, kc, ta:ta + tn],
                                    kc == 0, kc == 7, reads=[('wg', b)] + xr, writes=[bk(2 * q)])
                        for kc in range(8):
                            self.mm(pu[:, 0:tn], wu[b][:, kc, jj * 128:(jj + 1) * 128], X[:, kc, ta:ta + tn],
                                    kc == 0, kc == 7, reads=[('wu', b)] + xr, writes=[bk(2 * q + 1)])
                        P.op('act', lambda e, q=q, pg=pg, tn=tn: e.activation(out=sg[q][:, 0:tn], in_=pg[:, 0:tn], func=AF.Silu),
                             reads=[bk(2 * q)], writes=[('sg', q)])
                        P.op('dve', lambda e, q=q, pu=pu, j=j, ta=ta, tn=tn: e.tensor_tensor(
                            out=actT[:, j, ta:ta + tn], in0=sg[q][:, 0:tn], in1=pu[:, 0:tn], op=ALU.mult),
                            reads=[('sg', q), bk(2 * q + 1)], writes=[('actT', j, ti)])
            if half == 1 and e_ + 1 < NEXP:
                load_xgT(e_ + 1)
            for s_ in range(CAP // 128):
                q = cnts['dn'] % 2
                cnts['dn'] += 1
                ti = min(s_ // 4, 2)
                pa, pb = ps[4 + 2 * q], ps[5 + 2 * q]
                for dh, pt in enumerate((pa, pb)):
                    for j in range(14):
                        self.mm(pt[:, :], actT[:, j, s_ * 128:(s_ + 1) * 128], W[:, j, dh * 512:(dh + 1) * 512],
                                j == 0, j == 13, reads=[('actT', j, ti), ('wd', u % 2)], writes=[bk(4 + 2 * q + dh)])
                self.copy('act', ysb[q][:, 0:512], pa[:, :], reads=[bk(4 + 2 * q)], writes=[('ysb', q)])
                self.copy('dve', ysb[q][:, 512:1024], pb[:, :], reads=[bk(5 + 2 * q)], writes=[('ysb', q)])
                r0 = e_ * CAP + s_ * 128
                self.load('sp', Ygv[r0:r0 + 128, half, :], ysb[q], reads=[('ysb', q)], writes=[('Yg_d', u, s_)], key=('yst', q))
        P.barrier()
        A.off = mark2
        HT = NT // 2
        ogh = A.bf16(HT * 2 * 2048).rearrange("p (k d) -> p k d", k=HT * 2)
        xt = [A.f32(1024) for _ in range(3)]
        tsum = [[A.f32(1024) for _ in range(2)] for _ in range(3)]
        z = [A.f32(1024) for _ in range(3)]
        xn = z
        xo = [A.f32(1024) for _ in range(3)]
        small = [dict(st=A.f32(12), mv=A.f32(2), rs=A.f32(1), nmr=A.f32(1)) for _ in range(3)]
        defCA, defCB = [], []
        for n in range(NT):
            q = n % 3
            t0 = n * 128
            H = n // HT
            if defCB:
                defCB.pop(0)()
            if defCA:
                defCA.pop(0)()
            if n % HT == 0:
                def gat_fn(g, sem, H=H):
                    base = self.loop_cnt
                    with g.Fori(0, 2 * HT) as i:
                        g.tensor_copy(out=icur, in_=slots_i[:, bass.ts(i + 2 * HT * H, 1)]).then_inc(self.loop_sem, 1)
                        g.wait_ge(self.loop_sem, base + i + 1)
                        g.indirect_dma_start(out=ogh[:, i, :], out_offset=None, in_=Yg_d,
                                             in_offset=bass.IndirectOffsetOnAxis(ap=icur, axis=0),
                                             bounds_check=NSLOT, oob_is_err=False).then_inc(sem, 16)
                        g.wait_ge(sem, 16 * i + 16)
                    self.loop_cnt += 2 * HT
                P.dma('pool', gat_fn, reads=['icur'], writes=['ogh', 'icur'], key=('gatloop', H), count=2 * HT, raw=True)
            self.load('sp', xt[q], x3_d[t0:t0 + 128, :], writes=[('xt', q)], key=('xt', q))
            P.op('act', lambda e, q=q: e.activation(out=z[q], in_=xt[q], func=AF.Copy, scale=ALPHA), reads=[('xt', q)], writes=[('z', q)])
            r = 2 * (n % HT)
            t1, t2 = tsum[q][0], tsum[q][1]
            P.op('dve', lambda e, r=r, t1=t1: e.tensor_tensor(out=t1, in0=ogh[:, r, 0:1024], in1=ogh[:, r, 1024:2048], op=ALU.add),
                 reads=['ogh'], writes=[('t1', q)])
            P.op('dve', lambda e, r=r, t2=t2: e.tensor_tensor(out=t2, in0=ogh[:, r + 1, 0:1024], in1=ogh[:, r + 1, 1024:2048], op=ALU.add),
                 reads=['ogh'], writes=[('t2', q)])
            P.op('dve', lambda e, q=q, n=n, t1=t1: e.scalar_tensor_tensor(out=z[q], in0=t1, scalar=wts[:, n, 0:1], in1=z[q], op0=ALU.mult, op1=ALU.add),
                 reads=[('t1', q), ('z', q)], writes=[('z', q)])
            P.op('dve', lambda e, q=q, n=n, t2=t2: e.scalar_tensor_tensor(out=z[q], in0=t2, scalar=wts[:, n, 1:2], in1=z[q], op0=ALU.mult, op1=ALU.add),
                 reads=[('t2', q), ('z', q)], writes=[('z', q)])
            tmp = dict(small[q]); tmp['xn'] = xn[q]
            self.ln1(z[q], ('z', q), tmp, 'C3ln%d' % q)

            def pieceA(q=q, n=n, t0=t0, tmp=tmp):
                self.ln2a(z[q], ('z', q), tmp, 'C3ln%d' % q)

                def pieceB():
                    self.ln2b(g_bc, b_bc, xo[q], ('xo', q), tmp, 'C3ln%d' % q, mul_eng='dve')
                    self.load('act', out_d[t0:t0 + 128, :], xo[q], reads=[('xo', q)], writes=[('out', n)], key=('ost', q))
                defCB.append(pieceB)
            defCA.append(pieceA)
        while defCA or defCB:
            if defCB:
                defCB.pop(0)()
            if defCA:
                defCA.pop(0)()
        P.barrier()


def make_tables():
    import ml_dtypes
    bf = ml_dtypes.bfloat16
    t = {}
    inv = np.zeros((4, 512), np.float32)
    for g in range(4):
        w = 2 ** (g + 1)
        inv[g] = 1.0 / np.minimum(np.arange(512) + 1, w)
    t['t_invc'] = inv.reshape(1, 4 * 512)
    t['t_ident'] = np.eye(128, dtype=np.float32).astype(bf)
    t['t_ident32'] = np.eye(128, dtype=np.float32)
    t['t_triU'] = np.triu(np.ones((128, 128), np.float32), 1).astype(bf)
    t['t_ones'] = np.ones((128, 128), np.float32).astype(bf)
    t['t_ebase'] = (np.arange(8, dtype=np.float32) * CAP).reshape(1, 8)
    half = 32
    freqs = np.power(np.float32(10000.0), -np.arange(half, dtype=np.float32) / np.float32(half)).astype(np.float32)
    def cs_table(pos):
        ang = pos.astype(np.float32)[None, :] * freqs[:, None]
        cos = np.cos(ang).astype(np.float32); sin = np.sin(ang).astype(np.float32)
        return np.concatenate([cos, cos, -sin, sin], axis=0)
    t['t_cs'] = cs_table(np.arange(S))
    csc = np.zeros((128, 256), np.float32)
    csc[:, :255] = cs_table(16 * np.arange(255) + 31)
    t['t_csc'] = csc
    p = np.arange(128)[:, None]; u = np.arange(S)[None, :]
    t['t_B'] = np.where(16 * p + 31 <= u, 0.0, NEG).astype(np.float32).astype(bf)
    n_ = np.arange(128)[:, None]; t_ = np.arange(128)[None, :]
    lo = np.where(n_ <= t_, 0.0, NEG).astype(np.float32)
    hi = np.where(n_ > t_, 0.0, NEG).astype(np.float32)
    t['t_trilo4'] = np.tile(lo, (1, 4)).astype(bf)
    t['t_trihi4'] = np.tile(hi, (1, 4)).astype(bf)
    t['t_E'] = (np.arange(S)[None, :] // 64 == np.arange(64)[:, None]).astype(np.float32).astype(bf)
    fold = np.zeros((128, 64), np.float32); fold[np.arange(64), np.arange(64)] = 1; fold[np.arange(64) + 64, np.arange(64)] = 1
    t['t_fold'] = fold.astype(bf)
    tt = np.arange(S)[:, None]; jj = np.arange(64)[None, :]
    blk_t = tt // 64
    bvalid = jj <= blk_t
    forced = (jj == 0) | (jj == blk_t) | (jj == blk_t - 1)
    t['t_bonus'] = np.where(bvalid, 1000.0 * forced, -1.0).astype(np.float32)
    t['t_valid'] = bvalid.astype(np.float32)
    cstart = 16 * np.arange(255)[:, None]; sstart = 64 * np.arange(64)[None, :]
    ovl = np.zeros((256, 64), np.float32)
    ovl[:255] = ((cstart <= sstart + 63) & (cstart + 31 >= sstart)).astype(np.float32)
    t['t_ovl'] = ovl.astype(bf)
    return t


TABLE_SPECS = {
    't_invc': ([1, 2048], F32),
    't_ident': ([128, 128], BF16),
    't_ident32': ([128, 128], F32),
    't_triU': ([128, 128], BF16),
    't_ones': ([128, 128], BF16),
    't_ebase': ([1, 8], F32),
    't_cs': ([128, S], F32), 't_csc': ([128, 256], F32), 't_B': ([128, S], BF16),
    't_trilo4': ([128, 512], BF16), 't_trihi4': ([128, 512], BF16), 't_E': ([64, S], BF16), 't_fold': ([128, 64], BF16),
    't_bonus': ([S, 64], F32), 't_valid': ([S, 64], F32), 't_ovl': ([256, 64], BF16),
}

PHASE_INPUTS = {
    'A1': ['x', 'xT', 'pool_w', 'pool_scale', 'ln_g', 'ln_b', 't_invc', 't_ident'],
    'A2': ['ffn_w_gu', 'ffn_w_down', 'ln_g', 'ln_b', 't_ident'],
    'B': ['nsa_w_in', 'w_in_sw', 'nsa_pe_k', 'nsa_w1_k', 'nsa_w2_k', 'w2k_sw', 'nsa_pe_v', 'nsa_w1_v', 'nsa_w2_v', 'nsa_w_out', 'ln_g', 'ln_b',
          't_ident', 't_cs', 't_csc', 't_B', 't_trilo4', 't_trihi4', 't_E', 't_fold', 't_bonus', 't_valid', 't_ovl'],
    'C': ['moe_router', 'moe_w_gu', 'moe_w_down', 'ln_g', 'ln_b', 't_ident', 't_ident32', 't_triU', 't_ones', 't_ebase'],
}
INPUT_SPECS = {
    'x': ([S, D], F32), 'xT': ([8, 128, S], F32),
    'pool_w': ([4, 256, 256], F32), 'pool_scale': ([1, D], F32),
    'ln_g': ([2, 2, D], F32), 'ln_b': ([2, 2, D], F32),
    'ffn_w_gu': ([D, 2 * D_FF], F32), 'ffn_w_down': ([D_FF, D], F32),
    'nsa_w_in': ([D, IN_PROJ], F32), 'w_in_sw': ([D, 1536], F32), 'nsa_pe_k': ([32, 64], F32), 'nsa_w1_k': ([2048, 128], F32),
    'nsa_w2_k': ([128, 64], F32), 'w2k_sw': ([128, 64], F32), 'nsa_pe_v': ([32, 64], F32), 'nsa_w1_v': ([2048, 128], F32),
    'nsa_w2_v': ([128, 64], F32), 'nsa_w_out': ([D, D], F32),
    'moe_router': ([D, NEXP], F32), 'moe_w_gu': ([NEXP, D, 2 * DFE], F32), 'moe_w_down': ([NEXP, DFE, D], F32),
}
INPUT_SPECS.update(TABLE_SPECS)
MID = {
    'x1_d': ([S, D], F32, 'A1', ['A2']),
    'x1T_d': ([8, 128, S], BF16, 'A1', ['A2']),
    'x2_d': ([S, D], F32, 'A2', ['B']),
    'x2T_d': ([8, 128, S], BF16, 'A2', ['B']),
    'x3_d': ([S, D], F32, 'B', ['C']),
    'qT_d': ([16, 64, S], BF16, 'B', ['B']),
    'Xg_d': ([NEXP * CAP, D], BF16, 'C', ['C']),
    'Yg_d': ([NEXP * CAP + 1, 2 * D], BF16, 'C', ['C']),
    'out': ([S, D], F32, 'C', []),
}
ARENA_COLS = 51 * 1024 + 512


def swap_halves_cols(w, head=64):
    r, c = w.shape
    return np.ascontiguousarray(w.reshape(r, c // head, 2, head // 2)[:, :, ::-1, :].reshape(r, c))


def host_inputs(inputs, b, tabs):
    x = np.asarray(inputs['x'][b], dtype=np.float32)
    w_in = np.asarray(inputs['nsa_w_in'][0])
    d = {
        'x': np.ascontiguousarray(x),
        'xT': np.ascontiguousarray(x.T.reshape(8, 128, S)),
        'pool_w': np.asarray(inputs['pool_w'][0]), 'pool_scale': np.asarray(inputs['pool_scale']),
        'ln_g': np.asarray(inputs['ln_g']), 'ln_b': np.asarray(inputs['ln_b']),
        'ffn_w_gu': np.asarray(inputs['ffn_w_gu'][0]), 'ffn_w_down': np.asarray(inputs['ffn_w_down'][0]),
        'nsa_w_in': w_in,
        'w_in_sw': swap_halves_cols(np.concatenate([w_in[:, 0:1024], w_in[:, 1536:1792], w_in[:, 2048:2304]], axis=1)),
        'nsa_pe_k': np.asarray(inputs['nsa_pe_k'][0]), 'nsa_w1_k': np.asarray(inputs['nsa_w1_k'][0]),
        'nsa_w2_k': np.asarray(inputs['nsa_w2_k'][0]), 'w2k_sw': swap_halves_cols(np.asarray(inputs['nsa_w2_k'][0])),
        'nsa_pe_v': np.asarray(inputs['nsa_pe_v'][0]), 'nsa_w1_v': np.asarray(inputs['nsa_w1_v'][0]),
        'nsa_w2_v': np.asarray(inputs['nsa_w2_v'][0]), 'nsa_w_out': np.asarray(inputs['nsa_w_out'][0]),
        'moe_router': np.asarray(inputs['moe_router'][0]), 'moe_w_gu': np.asarray(inputs['moe_w_gu'][0]),
        'moe_w_down': np.asarray(inputs['moe_w_down'][0]),
    }
    d.update(tabs)
    return d


def build(phases, dump=(), debug=False):
    nc = bass.Bass("TRN2", target_bir_lowering=False)
    k = K(nc, phases, None)
    k.debug = debug
    need = []
    for ph in phases:
        for n in PHASE_INPUTS[ph]:
            if n not in need:
                need.append(n)
    for n in need:
        shp, dt = INPUT_SPECS[n]
        k.din(n, shp, dt)
    outs = []
    ins = list(need)
    for n, (shp, dt, prod, cons) in MID.items():
        p_here = prod in phases
        c_here = any(c in phases for c in cons)
        if not (p_here or c_here):
            continue
        if n in dump and p_here:
            c_here_eff = False
        else:
            c_here_eff = c_here
        if p_here and c_here and n in dump:
            t = nc.dram_tensor(n, list(shp), dt, kind="ExternalOutput")
            k.dram[n] = t.ap()
            outs.append(n)
        else:
            k.dmid(n, shp, dt, p_here, c_here_eff)
            if p_here and not c_here_eff:
                outs.append(n)
            elif c_here and not p_here:
                ins.append(n)
    with contextlib.ExitStack() as es:
        big = es.enter_context(nc.sbuf_tensor("arena", [128, ARENA_COLS], F32))
        A = Arena(big, ARENA_COLS)
        ps = [es.enter_context(nc.psum_tensor("ps%d" % i, [128, 512], F32)) for i in range(8)]
        k.loop_sem = es.enter_context(nc.semaphore('loopc'))
        k.loop_cnt = 0
        for ph in phases:
            getattr(k, 'phase_' + ph)(A, ps)
        k.P.emit()
    outs = outs + k.dbg_outs
    return nc, ins, outs


PHASES = ['A1', 'A2', 'B', 'C']
_CACHE = {}


def kernel(**inputs):
    tabs = make_tables()
    if 'nc' not in _CACHE:
        _CACHE['nc'] = build(PHASES)
    nc, ins, outs = _CACHE['nc']
    B = inputs['x'].shape[0]
    in_maps = []
    for b in range(B):
        hi = host_inputs(inputs, b, tabs)
        in_maps.append({n: hi[n] for n in ins})
    res = run_bass_kernel_spmd(nc, in_maps, core_ids=list(range(B)))
    out = np.stack([np.asarray(r['out'], dtype=np.float32) for r in res.results], axis=0)
    return out
```

```python
import contextlib
import numpy as np
import concourse.bass as bass
import concourse.mybir as mybir
from concourse.bass_utils import run_bass_kernel_spmd

F32 = mybir.dt.float32
BF16 = mybir.dt.bfloat16
I32 = mybir.dt.int32
AF = mybir.ActivationFunctionType
ALU = mybir.AluOpType

S = 4096
D = 1024
NT = S // 128
ALPHA = 4.0 ** 0.25
LN_EPS = 1e-5
D_FF = 2816
NEG = -30000.0
ATTN_SCALE = 0.125
NEXP = 8
DFE = 3584
CAP = 1280
IN_PROJ = 2608


class Prog:
    ENG = ['pe', 'act', 'dve', 'pool', 'sp']

    def __init__(self, nc):
        self.nc = nc
        self.streams = {e: [] for e in self.ENG}
        self.lastw = {}
        self.readers = {}
        self.dma_keys = {}

    def _add(self, eng, fn, reads, writes, dma_key=None):
        st = self.streams[eng]
        idx = len(st)
        me = (eng, idx)
        deps = set()
        for k in reads:
            w = self.lastw.get(k)
            if w is not None:
                deps.add(w)
        for k in writes:
            w = self.lastw.get(k)
            if w is not None:
                deps.add(w)
            for r in self.readers.get(k, {}).values():
                deps.add(r)
        deps.discard(me)
        rec = dict(fn=fn, deps=deps, dma_key=dma_key, signaled=False, eng=eng, idx=idx)
        if dma_key is not None:
            n = self.dma_keys.get(dma_key, 0) + 1
            self.dma_keys[dma_key] = n
            rec['dma_cnt'] = n
        st.append(rec)
        for k in reads:
            self.readers.setdefault(k, {})[eng if dma_key is None else (eng, dma_key)] = me
        for k in writes:
            self.lastw[k] = me
            self.readers[k] = {}
        return me

    def op(self, eng, fn, reads=(), writes=()):
        return self._add(eng, fn, list(reads), list(writes))

    def dma(self, eng, fn, reads=(), writes=(), key=None, count=1, raw=False):
        assert key is not None
        me = self._add(eng, fn, list(reads), list(writes), dma_key=key)
        rec = self.streams[eng][me[1]]
        rec['raw'] = raw
        if count > 1:
            self.dma_keys[key] += count - 1
            rec['dma_cnt'] += count - 1
        return me

    def barrier(self):
        tails = set()
        for e in self.ENG:
            st = self.streams[e]
            for r in reversed(st):
                if r['fn'] is not None and r['dma_key'] is None:
                    tails.add((e, r['idx']))
                    break
        lastdma = {}
        for e in self.ENG:
            for r in self.streams[e]:
                if r['dma_key'] is not None:
                    lastdma[r['dma_key']] = (e, r['idx'])
        alld = tails | set(lastdma.values())
        for e in self.ENG:
            self.streams[e].append(dict(fn=None, deps=set(alld), dma_key=None, signaled=False,
                                        eng=e, idx=len(self.streams[e])))
        self.lastw = {}
        self.readers = {}

    def emit(self):
        nc = self.nc
        for e in self.ENG:
            for r in self.streams[e]:
                drop = set()
                for (de, di) in r['deps']:
                    d = self.streams[de][di]
                    if d['fn'] is None:
                        drop.add((de, di))
                        continue
                    if de == 'pe' and e == 'pe' and d['dma_key'] is None and r['fn'] is not None:
                        drop.add((de, di))
                        continue
                    d['signaled'] = True
                r['deps'] -= drop
        for e in self.ENG:
            c = 0
            for r in self.streams[e]:
                if r['dma_key'] is None and r['signaled']:
                    c += 1
                    r['cnt'] = c
        with contextlib.ExitStack() as es:
            esem = {e: es.enter_context(nc.semaphore('s_' + e)) for e in self.ENG}
            dsem = {}
            for k in self.dma_keys:
                dsem[k] = es.enter_context(nc.semaphore('d_%d' % len(dsem)))
            block = es.enter_context(nc.Block())
            engobj = {'pe': 'tensor', 'act': 'scalar', 'dve': 'vector', 'pool': 'gpsimd', 'sp': 'sync'}

            def make(e):
                def body(eng):
                    seen = {}
                    for r in self.streams[e]:
                        for (de, di) in sorted(r['deps']):
                            d = self.streams[de][di]
                            if d['dma_key'] is not None:
                                sem = dsem[d['dma_key']]
                                val = 16 * d['dma_cnt']
                            else:
                                sem = esem[de]
                                val = d['cnt']
                            key = id(sem)
                            if seen.get(key, 0) >= val:
                                continue
                            seen[key] = val
                            eng.wait_ge(sem, val)
                        if r['fn'] is None:
                            continue
                        if r.get('raw'):
                            r['fn'](eng, dsem[r['dma_key']])
                            continue
                        ins = r['fn'](eng)
                        if r['dma_key'] is not None:
                            ins.then_inc(dsem[r['dma_key']], 16)
                        elif r['signaled']:
                            ins.then_inc(esem[e], 1)
                return body
            for e in self.ENG:
                getattr(block, engobj[e])(make(e))


class Arena:
    def __init__(self, big, ncols):
        self.big = big
        self.ncols = ncols
        self.off = 0
        self.mark_ = 0

    def f32(self, cols):
        a = self.off
        self.off += cols
        assert self.off <= self.ncols, ("SBUF arena overflow", self.off, self.ncols)
        return self.big[:, a:a + cols]

    def bf16(self, cols):
        c4 = (cols + 1) // 2
        return self.f32(c4).bitcast(BF16)[:, 0:cols]

    def i32(self, cols):
        return self.f32(cols).bitcast(I32)

    def mark(self):
        self.mark_ = self.off

    def reset(self):
        self.off = self.mark_


class K:
    def __init__(self, nc, phases, ext):
        self.nc = nc
        self.P = Prog(nc)
        self.phases = phases
        self.ext = ext
        self.dram = {}
        self.rr = 0
        self.debug = False
        self.dbg_outs = []

    def din(self, name, shape, dt=F32):
        t = self.nc.dram_tensor(name, list(shape), dt, kind="ExternalInput")
        self.dram[name] = t.ap()
        return self.dram[name]

    def dmid(self, name, shape, dt, produced_here, consumed_here):
        if produced_here and consumed_here:
            t = self.nc.dram_tensor(name, list(shape), dt)
        elif produced_here:
            t = self.nc.dram_tensor(name, list(shape), dt, kind="ExternalOutput")
        else:
            t = self.nc.dram_tensor(name, list(shape), dt, kind="ExternalInput")
        self.dram[name] = t.ap()
        return self.dram[name]

    def dbg(self, name, ap, shape, dt, reads):
        if not getattr(self, 'debug', False):
            return
        t = self.nc.dram_tensor(name, list(shape), dt, kind="ExternalOutput")
        self.dbg_outs.append(name)
        self.load('sp', t.ap(), ap, reads=reads, writes=[('dbg', name)], key='dbg')

    def load(self, eng, out, in_, reads=(), writes=(), key=None):
        cg = getattr(self, '_cg', None)
        if cg is not None and isinstance(key, str) and key.startswith('c_') or (cg is not None and isinstance(key, tuple) and isinstance(key[0], str) and key[0].startswith('c_')):
            me = self.P.dma(eng, lambda e: e.dma_start(out=out, in_=in_), reads=reads, writes=writes, key=cg['key'])
            cg['last'] = me
            cg['writes'].extend(list(writes))
            return
        self.P.dma(eng, lambda e: e.dma_start(out=out, in_=in_), reads=reads, writes=writes, key=key)

    def cg_begin(self, key):
        self._cg = dict(key=key, last=None, writes=[])

    def cg_end(self):
        cg = self._cg
        self._cg = None
        if cg['last'] is not None:
            for w in cg['writes']:
                self.P.lastw[w] = cg['last']

    def evac_eng(self):
        self.rr += 1
        return 'act' if self.rr % 2 else 'dve'

    def copy(self, eng, out, in_, reads, writes):
        if eng == 'act':
            self.P.op('act', lambda e: e.activation(out=out, in_=in_, func=AF.Copy), reads=reads, writes=writes)
        else:
            self.P.op(eng, lambda e: e.tensor_copy(out=out, in_=in_), reads=reads, writes=writes)

    def mm(self, out, lhsT, rhs, start, stop, reads, writes):
        self.P.op('pe', lambda e: e.matmul(out, lhsT=lhsT, rhs=rhs, start=start, stop=stop), reads=reads, writes=writes)

    def layernorm(self, z, zk, gbc, bbc, xo, xok, tmp, tk, mul_eng='pool'):
        self.ln1(z, zk, tmp, tk)
        self.ln2(z, zk, gbc, bbc, xo, xok, tmp, tk, mul_eng)

    def ln1(self, z, zk, tmp, tk):
        P = self.P
        st, mv, rs, nmr = tmp['st'], tmp['mv'], tmp['rs'], tmp['nmr']
        P.op('dve', lambda e: e.bn_stats(out=st[:, 0:6], in_=z[:, 0:512]), reads=[zk], writes=[tk + 'st0'])
        P.op('dve', lambda e: e.bn_stats(out=st[:, 6:12], in_=z[:, 512:1024]), reads=[zk], writes=[tk + 'st1'])
        P.op('dve', lambda e: e.bn_aggr(out=mv[:, 0:2], in_=st[:, 0:12].rearrange("p (a b) -> p a b", a=2)),
             reads=[tk + 'st0', tk + 'st1'], writes=[tk + 'mv'])
        P.op('act', lambda e: e.activation(out=rs[:, 0:1], in_=mv[:, 1:2], func=AF.Sqrt, bias=LN_EPS, scale=1.0),
             reads=[tk + 'mv'], writes=[tk + 'rs'])

    def ln2(self, z, zk, gbc, bbc, xo, xok, tmp, tk, mul_eng='pool'):
        self.ln2a(z, zk, tmp, tk)
        self.ln2b(gbc, bbc, xo, xok, tmp, tk, mul_eng)

    def ln2a(self, z, zk, tmp, tk):
        P = self.P
        st, mv, rs, nmr = tmp['st'], tmp['mv'], tmp['rs'], tmp['nmr']
        P.op('dve', lambda e: e.reciprocal(out=rs[:, 0:1], in_=rs[:, 0:1]), reads=[tk + 'rs'], writes=[tk + 'rs'])
        P.op('dve', lambda e: e.scalar_tensor_tensor(out=nmr[:, 0:1], in0=mv[:, 0:1], scalar=-1.0, in1=rs[:, 0:1],
                                                      op0=ALU.mult, op1=ALU.mult), reads=[tk + 'mv', tk + 'rs'], writes=[tk + 'nmr'])
        xn = tmp['xn']
        P.op('act', lambda e: e.activation(out=xn, in_=z, func=AF.Identity, bias=nmr[:, 0:1], scale=rs[:, 0:1]),
             reads=[zk, tk + 'rs', tk + 'nmr'], writes=[tk + 'xn'])

    def ln2b(self, gbc, bbc, xo, xok, tmp, tk, mul_eng='pool'):
        P = self.P
        xn = tmp['xn']
        P.op(mul_eng, lambda e: e.tensor_tensor(out=xn, in0=xn, in1=gbc, op=ALU.mult), reads=[tk + 'xn'], writes=[tk + 'xn'])
        P.op('dve', lambda e: e.tensor_tensor(out=xo, in0=xn, in1=bbc, op=ALU.add), reads=[tk + 'xn'], writes=[xok])

    def phase_A1(self, A, ps):
        P, nc, dr = self.P, self.nc, self.dram
        A.reset()
        xT_d, x_d = dr['xT'], dr['x']
        x1_d, x1T_d = dr['x1_d'], dr['x1T_d']
        xh = [A.f32(8 * 528).rearrange("p (c t) -> p c t", c=8) for _ in range(2)]
        sA = A.f32(2 * 528).rearrange("p (c t) -> p c t", c=2)
        sB = A.f32(2 * 528).rearrange("p (c t) -> p c t", c=2)
        diffT = [A.bf16(8 * 512).rearrange("p (c t) -> p c t", c=8) for _ in range(2)]
        poolw = A.bf16(4 * 2 * 256).rearrange("p (g k d) -> p g k d", g=4, k=2)
        scale_bc, g_bc, b_bc = A.f32(1024), A.f32(1024), A.f32(1024)
        invc = A.f32(4 * 512).rearrange("p (g t) -> p g t", g=4)
        ident = A.bf16(128)
        NQ = 3
        xtok = [A.f32(1024) for _ in range(NQ)]
        hs = [A.f32(1024) for _ in range(NQ)]
        z = [A.f32(1024) for _ in range(NQ)]
        xn = [A.f32(1024) for _ in range(NQ)]
        xo = [A.f32(1024) for _ in range(NQ)]
        xb = [A.bf16(1024) for _ in range(NQ)]
        xst = [A.bf16(8 * 512).rearrange("p (c t) -> p c t", c=8) for _ in range(2)]
        small = [dict(st=A.f32(12), mv=A.f32(2), rs=A.f32(1), nmr=A.f32(1)) for _ in range(NQ)]
        self.cg_begin('cgA1')
        self.load('pool', poolw, dr['pool_w'].rearrange("g (k p) d -> p g k d", p=128), writes=['poolw'], key='c_poolw')
        self.load('sp', scale_bc, dr['pool_scale'][0].partition_broadcast(128), writes=['scale_bc'], key='c_scale')
        self.load('sp', g_bc, dr['ln_g'][0, 0].partition_broadcast(128), writes=['g_bc'], key='c_g')
        self.load('sp', b_bc, dr['ln_b'][0, 0].partition_broadcast(128), writes=['b_bc'], key='c_b')
        self.load('sp', invc, dr['t_invc'][0].partition_broadcast(128).rearrange("p (g t) -> p g t", g=4), writes=['invc'], key='c_invc')
        self.load('sp', ident, dr['t_ident'], writes=['ident'], key='c_ident')
        self.cg_end()
        defA, defB = [], []
        for i in range(8):
            T0 = i * 512
            X = xh[i % 2]
            xk = ('xh', i % 2)
            if i == 0:
                P.op('pool', lambda e, X=X: e.memset(X[:, :, 0:16], 0.0), writes=[xk])
                self.load('sp', X[:, :, 16:528], xT_d[:, :, 0:512].rearrange("c p t -> p c t"), writes=[xk], key=('xh', 0))
            else:
                self.load('sp', X, xT_d[:, :, T0 - 16:T0 + 512].rearrange("c p t -> p c t"), writes=[xk], key=('xh', i % 2))
            DT = diffT[i % 2]
            dk = ('diffT', i % 2)
            for g in range(4):
                w = 2 ** (g + 1)
                xg = X[:, 2 * g:2 * g + 2, :]
                src, srck = xg, xk
                bufs = [(sA, 'sA'), (sB, 'sB')]
                sh = 1
                lo = 1
                for stp in range(g + 1):
                    dst, dstk = bufs[stp % 2]
                    P.op('dve', lambda e, dst=dst, src=src, lo=lo, sh=sh: e.tensor_tensor(
                        out=dst[:, :, lo:528], in0=src[:, :, lo:528], in1=src[:, :, lo - sh:528 - sh], op=ALU.add),
                        reads=[srck], writes=[dstk])
                    src, srck = dst, dstk
                    sh *= 2
                    lo += sh
                if i == 0:
                    P.op('dve', lambda e, src=src, g=g: e.tensor_tensor(
                        out=src[:, :, 16:528], in0=src[:, :, 16:528],
                        in1=invc[:, g:g + 1, :].to_broadcast([128, 2, 512]), op=ALU.mult), reads=[srck, 'invc'], writes=[srck])
                    P.op('dve', lambda e, src=src, xg=xg, DT=DT, g=g: e.tensor_tensor(
                        out=DT[:, 2 * g:2 * g + 2, :], in0=src[:, :, 16:528], in1=xg[:, :, 16:528], op=ALU.subtract),
                        reads=[srck, xk], writes=[dk])
                else:
                    P.op('dve', lambda e, src=src, xg=xg, DT=DT, g=g, w=w: e.scalar_tensor_tensor(
                        out=DT[:, 2 * g:2 * g + 2, :], in0=src[:, :, 16:528], scalar=1.0 / w, in1=xg[:, :, 16:528],
                        op0=ALU.mult, op1=ALU.subtract), reads=[srck, xk], writes=[dk])
            XS = xst[i % 2]
            xsk = ('xst', i % 2)
            for s in range(4):
                n = i * 4 + s
                q = n % NQ
                t0 = T0 + s * 128
                if defB:
                    defB.pop(0)()
                if defA:
                    defA.pop(0)()
                pa, pb = ps[2 * q], ps[2 * q + 1]
                pk = ('psA', q)
                self.load('sp', xtok[q], x_d[t0:t0 + 128, :], writes=[('xtok', q)], key=('xtok', q))
                for g in range(4):
                    pt = pa if g < 2 else pb
                    for kc in range(2):
                        self.mm(pt[:, (g % 2) * 256:(g % 2) * 256 + 256], DT[:, 2 * g + kc, s * 128:(s + 1) * 128],
                                poolw[:, g, kc, :], kc == 0, kc == 1, reads=[dk, 'poolw'], writes=[pk])
                P.op('dve', lambda e, q=q, pa=pa: e.tensor_tensor(out=hs[q][:, 0:512], in0=pa[:, :], in1=scale_bc[:, 0:512], op=ALU.mult),
                     reads=[pk, 'scale_bc'], writes=[('hs', q)])
                P.op('dve', lambda e, q=q, pb=pb: e.tensor_tensor(out=hs[q][:, 512:1024], in0=pb[:, :], in1=scale_bc[:, 512:1024], op=ALU.mult),
                     reads=[pk, 'scale_bc'], writes=[('hs', q)])
                P.op('dve', lambda e, q=q: e.scalar_tensor_tensor(out=z[q], in0=xtok[q], scalar=ALPHA, in1=hs[q], op0=ALU.mult, op1=ALU.add),
                     reads=[('xtok', q), ('hs', q)], writes=[('z', q)])
                tmp = dict(small[q]); tmp['xn'] = xn[q]
                self.ln1(z[q], ('z', q), tmp, 'A1ln%d' % q)

                def pieceA(q=q, n=n, t0=t0, s=s, i=i, XS=XS, xsk=xsk, tmp=tmp, T0=T0):
                    self.ln2(z[q], ('z', q), g_bc, b_bc, xo[q], ('xo', q), tmp, 'A1ln%d' % q)
                    self.load('act', x1_d[t0:t0 + 128, :], xo[q], reads=[('xo', q)], writes=[('x1_d', n)], key=('x1st', q))
                    self.copy('act', xb[q], xo[q], reads=[('xo', q)], writes=[('xb', q)])

                    def pieceB():
                        tb = ps[6 + n % 2][:, :].bitcast(BF16)
                        tk = ('pst', n % 2)
                        for c in range(8):
                            P.op('pe', lambda e, c=c: e.transpose(tb[:, c * 128:(c + 1) * 128], xb[q][:, c * 128:(c + 1) * 128], ident),
                                 reads=[('xb', q), 'ident'], writes=[tk])
                        self.copy('act', XS[:, :, s * 128:(s + 1) * 128], tb.rearrange("p (c t) -> p c t", c=8), reads=[tk], writes=[xsk])
                        if s == 3:
                            self.load('act', x1T_d[:, :, T0:T0 + 512].rearrange("c p t -> p c t"), XS, reads=[xsk], writes=[('x1T_d', i)], key=('xst', i % 2))
                    defB.append(pieceB)
                defA.append(pieceA)
        while defA or defB:
            if defB:
                defB.pop(0)()
            if defA:
                defA.pop(0)()
        P.barrier()

    def phase_A2(self, A, ps):
        P, nc, dr = self.P, self.nc, self.dram
        A.reset()
        x1_d, x1T_d, x2_d, x2T_d = dr['x1_d'], dr['x1T_d'], dr['x2_d'], dr['x2T_d']
        wgu, wdn = dr['ffn_w_gu'], dr['ffn_w_down']
        x1T = A.bf16(8 * 1024).rearrange("p (c t) -> p c t", c=8)
        wg = [A.bf16(8 * 512).rearrange("p (c f) -> p c f", c=8) for _ in range(2)]
        wu = [A.bf16(8 * 512).rearrange("p (c f) -> p c f", c=8) for _ in range(2)]
        actT = A.bf16(22 * 1024).rearrange("p (j t) -> p j t", j=22)
        wd = A.bf16(22 * 1024).rearrange("p (j d) -> p j d", j=22)
        g_bc, b_bc = A.f32(1024), A.f32(1024)
        ident = A.bf16(128)
        sg = [A.f32(512) for _ in range(2)]
        NQ = 3
        xtok = [A.f32(1024) for _ in range(NQ)]
        z = [A.f32(1024) for _ in range(NQ)]
        xn = z
        xo = [A.f32(1024) for _ in range(NQ)]
        xb = [A.bf16(1024) for _ in range(NQ)]
        xst = [A.bf16(8 * 128).rearrange("p (c t) -> p c t", c=8) for _ in range(NQ)]
        small = [dict(st=A.f32(12), mv=A.f32(2), rs=A.f32(1), nmr=A.f32(1)) for _ in range(NQ)]
        self.cg_begin('cgA2')
        self.load('sp', g_bc, dr['ln_g'][0, 1].partition_broadcast(128), writes=['g_bc'], key='c_g')
        self.load('sp', b_bc, dr['ln_b'][0, 1].partition_broadcast(128), writes=['b_bc'], key='c_b')
        self.load('sp', ident, dr['t_ident'], writes=['ident'], key='c_ident')
        self.cg_end()
        wdv = wdn.rearrange("(j p) d -> p j d", p=128)
        wguv = wgu.rearrange("(c p) f -> p c f", p=128)
        nblk = 6
        cnt = 0
        gu = 0
        defA, defB = [], []
        for st_ in range(4):
            T0 = st_ * 1024
            self.load('sp', x1T, x1T_d[:, :, T0:T0 + 1024].rearrange("c p t -> p c t"), writes=['x1T'], key='x1T')
            for fb in range(nblk):
                nch = 4 if fb < 5 else 2
                b = cnt % 2
                cnt += 1
                self.load('pool', wg[b][:, :, 0:nch * 128], wguv[:, :, fb * 512:fb * 512 + nch * 128], writes=[('wg', b)], key=('wg', b))
                self.load('pool', wu[b][:, :, 0:nch * 128], wguv[:, :, D_FF + fb * 512:D_FF + fb * 512 + nch * 128], writes=[('wu', b)], key=('wu', b))
                if st_ == 0 and fb == 1:
                    for j0 in range(0, 22, 6):
                        j1 = min(22, j0 + 6)
                        self.load('pool', wd[:, j0:j1, :], wdv[:, j0:j1, :], writes=[('wd', j0)], key=('wd', j0))
                for jj in range(nch):
                    j = fb * 4 + jj
                    for hf in range(2):
                        q = gu % 2
                        gu += 1
                        pg, pu = ps[2 * q], ps[2 * q + 1]
                        for kc in range(8):
                            self.mm(pg[:, :], wg[b][:, kc, jj * 128:(jj + 1) * 128], x1T[:, kc, hf * 512:(hf + 1) * 512],
                                    kc == 0, kc == 7, reads=[('wg', b), 'x1T'], writes=[('pg', q)])
                        for kc in range(8):
                            self.mm(pu[:, :], wu[b][:, kc, jj * 128:(jj + 1) * 128], x1T[:, kc, hf * 512:(hf + 1) * 512],
                                    kc == 0, kc == 7, reads=[('wu', b), 'x1T'], writes=[('pu', q)])
                        P.op('act', lambda e, q=q, pg=pg: e.activation(out=sg[q], in_=pg[:, :], func=AF.Silu),
                             reads=[('pg', q)], writes=[('sg', q)])
                        P.op('dve', lambda e, q=q, pu=pu, j=j, hf=hf: e.tensor_tensor(
                            out=actT[:, j, hf * 512:(hf + 1) * 512], in0=sg[q], in1=pu[:, :], op=ALU.mult),
                            reads=[('sg', q), ('pu', q)], writes=[('actT', j, hf)])
            for s in range(8):
                n = st_ * 8 + s
                qb = n % 2
                q = n % NQ
                t0 = T0 + s * 128
                if defB:
                    defB.pop(0)()
                if defA:
                    defA.pop(0)()
                self.load('sp', xtok[q], x1_d[t0:t0 + 128, :], reads=[('x1_d', n)], writes=[('xtok', q)], key=('xtok', q))
                pa, pb = ps[4 + 2 * qb], ps[5 + 2 * qb]
                pk = ('pdn', qb)
                for dh, pt in enumerate((pa, pb)):
                    for j in range(22):
                        self.mm(pt[:, :], actT[:, j, s * 128:(s + 1) * 128], wd[:, j, dh * 512:(dh + 1) * 512],
                                j == 0, j == 21, reads=[('actT', j, s // 4), ('wd', (j // 6) * 6)], writes=[pk])
                P.op('dve', lambda e, q=q, pa=pa: e.scalar_tensor_tensor(out=z[q][:, 0:512], in0=xtok[q][:, 0:512], scalar=ALPHA, in1=pa[:, :],
                                                                          op0=ALU.mult, op1=ALU.add), reads=[('xtok', q), pk], writes=[('z', q)])
                P.op('dve', lambda e, q=q, pb=pb: e.scalar_tensor_tensor(out=z[q][:, 512:1024], in0=xtok[q][:, 512:1024], scalar=ALPHA, in1=pb[:, :],
                                                                          op0=ALU.mult, op1=ALU.add), reads=[('xtok', q), pk], writes=[('z', q)])
                tmp = dict(small[q]); tmp['xn'] = xn[q]
                self.ln1(z[q], ('z', q), tmp, 'A2ln%d' % q)

                def pieceA(q=q, n=n, t0=t0, tmp=tmp):
                    self.ln2(z[q], ('z', q), g_bc, b_bc, xo[q], ('xo', q), tmp, 'A2ln%d' % q)
                    self.load('act', x2_d[t0:t0 + 128, :], xo[q], reads=[('xo', q)], writes=[('x2_d', n)], key=('x2st', q))
                    self.copy('act', xb[q], xo[q], reads=[('xo', q)], writes=[('xb', q)])

                    def pieceB():
                        tbk = ('pg', 0) if n % 2 == 0 else ('pu', 0)
                        tb = ps[n % 2][:, :].bitcast(BF16)
                        for c in range(8):
                            P.op('pe', lambda e, c=c: e.transpose(tb[:, c * 128:(c + 1) * 128], xb[q][:, c * 128:(c + 1) * 128], ident),
                                 reads=[('xb', q), 'ident'], writes=[tbk])
                        self.copy('act', xst[q], tb.rearrange("p (c t) -> p c t", c=8), reads=[tbk], writes=[('xst', q)])
                        self.load('act', x2T_d[:, :, t0:t0 + 128].rearrange("c p t -> p c t"), xst[q], reads=[('xst', q)], writes=[('x2T_d', n)], key=('xst', q))
                    defB.append(pieceB)
                defA.append(pieceA)
        while defA or defB:
            if defB:
                defB.pop(0)()
            if defA:
                defA.pop(0)()
        P.barrier()

    def phase_B(self, A, ps):
        P, nc, dr = self.P, self.nc, self.dram
        A.reset()
        x2_d, x2T_d, x3_d, qT_d = dr['x2_d'], dr['x2T_d'], dr['x3_d'], dr['qT_d']
        w_in, w_sw = dr['nsa_w_in'], dr['w_in_sw']
        bk = lambda i: ('bank', i)
        ksT = A.bf16(4 * S).rearrange("p (g t) -> p g t", g=4)
        kwT = A.bf16(4 * S).rearrange("p (g t) -> p g t", g=4)
        vs_aug = A.bf16(NT * 4 * 66).rearrange("p (n g d) -> p n g d", n=NT, g=4)
        vw_aug = A.bf16(NT * 4 * 66).rearrange("p (n g d) -> p n g d", n=NT, g=4)
        kcT = A.bf16(4 * 256).rearrange("p (g n) -> p g n", g=4)
        vc_aug = A.bf16(2 * 4 * 130).rearrange("p (c g d) -> p c g d", c=2, g=4)
        gates = A.f32(NT * 48).rearrange("p (n k) -> p n k", n=NT)
        ident = A.bf16(128)
        fold = A.bf16(64)
        self.cg_begin('cgB0')
        self.load('sp', ident, dr['t_ident'], writes=['ident'], key='c_ident')
        self.load('sp', fold, dr['t_fold'], writes=['fold'], key='c_fold')
        for g in range(4):
            self.load('sp', ksT[64:128, g, :], dr['t_E'], writes=[('ksE', g)], key=('c_E', g))
        self.cg_end()
        P.op('pool', lambda e: e.memset(vc_aug, 0.0), writes=['vc_aug0'])
        P.op('pool', lambda e: e.memset(vs_aug[:, :, :, 64:65], 1.0), writes=['vs_one'])
        P.op('pool', lambda e: e.memset(vw_aug[:, :, :, 64:65], 1.0), writes=['vw_one'])
        P.op('pool', lambda e: e.memset(kcT, 0.0), writes=['kcT0'])
        markP = A.off
        x2T = A.bf16(8 * S).rearrange("p (c t) -> p c t", c=8)
        for c in range(8):
            self.load('sp', x2T[:, c, :], x2T_d[c], writes=[('x2T', c)], key=('x2T', c))
        x2r = [('x2T', c) for c in range(8)]
        markX = A.off
        kvcr = kwT
        wblk = [A.bf16(8 * 128).rearrange("p (c f) -> p c f", c=8) for _ in range(2)]
        w1pad = A.bf16(2 * 32 * 128).rearrange("p (k l j) -> p k l j", k=2, l=32)
        pecol = A.bf16(64)
        penat = A.bf16(128)
        w2kk = A.bf16(128)
        w2v = A.bf16(64)
        csc = A.f32(256)
        biasb = A.f32(2)
        hid = [A.bf16(256) for _ in range(2)]
        Tb = [A.bf16(512) for _ in range(2)]
        P.op('pool', lambda e: e.memset(w1pad, 0.0), writes=['w1pad'])
        self.cg_begin('cgB1')
        self.load('pool', w1pad[0:64, 0, :, :], dr['nsa_w1_k'].rearrange("(l d) j -> d l j", d=64), reads=[], writes=['w1pad'], key='c_w1k')
        self.load('pool', w1pad[0:64, 1, :, :], dr['nsa_w1_v'].rearrange("(l d) j -> d l j", d=64), reads=['w1pad'], writes=['w1pad_v'], key='c_w1v')
        self.load('pool', penat[0:32, 0:64], dr['nsa_pe_k'], writes=['pen_k'], key='c_pek')
        self.load('pool', penat[0:32, 64:128], dr['nsa_pe_v'], writes=['pen_v'], key='c_pev')
        self.cg_end()
        tbp = ps[3][:, :].bitcast(BF16)
        for kv in range(2):
            P.op('pe', lambda e, kv=kv: e.transpose(tbp[0:64, kv * 32:(kv + 1) * 32], penat[0:32, kv * 64:(kv + 1) * 64], ident[0:32, 0:32]),
                 reads=['pen_k', 'pen_v', 'ident'], writes=[bk(3)])
        self.copy('dve', pecol[0:64, :], tbp[0:64, 0:64], reads=[bk(3)], writes=['pe_k', 'pe_v'])
        self.cg_begin('cgB1b')
        self.load('pool', w2kk[:, 0:64], dr['nsa_w2_k'], writes=['w2k_a'], key='c_w2k')
        self.load('pool', w2kk[:, 64:128], dr['w2k_sw'], writes=['w2k_b'], key='c_w2ks')
        self.load('pool', w2v, dr['nsa_w2_v'], writes=['w2v'], key='c_w2v')
        self.load('sp', csc, dr['t_csc'], writes=['csc'], key='c_csc')
        self.cg_end()
        nb = 0
        for g in range(4):
            b = nb % 2
            nb += 1
            self.load('pool', wblk[b][:, :, 0:64], w_in[:, 1024 + g * 64:1024 + (g + 1) * 64].rearrange("(c p) f -> p c f", p=128),
                      writes=[('wblk', b, 0)], key=('wblk', b, 0))
            self.load('pool', wblk[b][:, :, 64:128], w_in[:, 1280 + g * 64:1280 + (g + 1) * 64].rearrange("(c p) f -> p c f", p=128),
                      writes=[('wblk', b, 1)], key=('wblk', b, 1))
            for ti in range(8):
                for kv in range(2):
                    q = kv
                    dstT = kwT if kv == 0 else ksT
                    for c in range(8):
                        self.mm(ps[q][0:64, :], wblk[b][:, c, kv * 64:(kv + 1) * 64], x2T[:, c, ti * 512:(ti + 1) * 512], c == 0, c == 7,
                                reads=[('wblk', b, kv), ('x2T', c)], writes=[bk(q)])
                    self.copy('act' if kv == 0 else 'dve', dstT[0:64, g, ti * 512:(ti + 1) * 512], ps[q][0:64, :], reads=[bk(q)], writes=[('kvcr', kv, g, ti)])
        for kv in range(2):
            for l in range(32):
                self.mm(ps[2][:, kv:kv + 1], w1pad[0:64, kv, l, :], pecol[0:64, kv * 32 + l:kv * 32 + l + 1], l == 0, l == 31,
                        reads=['w1pad', 'w1pad_v', 'pe_k', 'pe_v'], writes=[bk(2)])
        self.copy('dve', biasb, ps[2][:, 0:2], reads=[bk(2)], writes=['biasb'])
        kvr = lambda g, kv: [('kvcr', kv, g, ti) for ti in range(8)]
        for g in range(4):
            for kv in range(2):
                q = kv
                pb_ = ps[4 + q]
                for l in range(32):
                    srcT = kwT if kv == 0 else ksT
                    self.mm(pb_[:, 0:255], w1pad[0:64, kv, l, :], srcT[0:64, g, l:l + 16 * 254 + 1:16], l == 0, l == 31,
                            reads=['w1pad', 'w1pad_v'] + kvr(g, kv), writes=[bk(4 + q)])
                P.op('act', lambda e, q=q, pb_=pb_, kv=kv: e.activation(out=hid[q][:, 0:255], in_=pb_[:, 0:255], func=AF.Silu, bias=biasb[:, kv:kv + 1]),
                     reads=[bk(4 + q), 'biasb'], writes=[('hid', q)])
                if g == 3 and getattr(self, 'debug', False):
                    dbt = A.f32(256)
                    self.copy('dve', dbt[:, 0:255], pb_[:, 0:255], reads=[bk(4 + q), ('hid', q)], writes=[('dbt', kv)])
                    self.dbg('dbg_pre%d' % kv, dbt, [128, 256], F32, [('dbt', kv)])
                if kv == 0:
                    self.mm(ps[6][:, 0:255], w2kk, hid[0][:, 0:255], True, True, reads=['w2k_a', 'w2k_b', ('hid', 0)], writes=[bk(6)])
                    P.op('dve', lambda e: e.tensor_tensor(out=Tb[0][:, 0:255], in0=ps[6][:, 0:255], in1=csc[:, 0:255], op=ALU.mult),
                         reads=[bk(6), 'csc'], writes=[('Tb', 0)])
                    self.mm(ps[7][0:64, 0:255], fold, Tb[0][:, 0:255], True, True, reads=['fold', ('Tb', 0)], writes=[bk(7)])
                    self.copy('act', kcT[0:64, g, 0:255], ps[7][0:64, 0:255], reads=[bk(7), 'kcT0'], writes=[('kcT', g)])
                else:
                    for c in range(2):
                        nn = 128 if c == 0 else 127
                        self.mm(ps[6][0:nn, 256 + c * 64:256 + (c + 1) * 64], hid[1][:, c * 128:c * 128 + nn], w2v, True, True,
                                reads=[('hid', 1), 'w2v'], writes=[bk(6)])
                        self.copy('dve', vc_aug[0:nn, c, g, 0:64], ps[6][0:nn, 256 + c * 64:256 + (c + 1) * 64],
                                  reads=[bk(6), 'vc_aug0'], writes=[('vc_aug', g)])
        self.dbg('dbg_vcr3', ksT[0:64, 3, :], [64, 4096], BF16, kvr(3, 1))
        self.dbg('dbg_kcr3', kwT[0:64, 3, :], [64, 4096], BF16, kvr(3, 0))
        self.dbg('dbg_w1v', w1pad[0:64, 1, :, :].rearrange("p l j -> p (l j)"), [64, 4096], BF16, ['w1pad', 'w1pad_v'])
        self.dbg('dbg_hid0', hid[0], [128, 256], BF16, [('hid', 0)])
        self.dbg('dbg_hid1', hid[1], [128, 256], BF16, [('hid', 1)])
        self.dbg('dbg_w1pa', w1pad[:, 0, 5, :], [128, 128], BF16, ['w1pad', 'w1pad_v'])
        self.dbg('dbg_w1pb', w1pad[:, 1, 5, :], [128, 128], BF16, ['w1pad', 'w1pad_v'])
        for l_ in (0, 17, 31):
            self.dbg('dbg_w1pb%d' % l_, w1pad[:, 1, l_, :], [128, 128], BF16, ['w1pad', 'w1pad_v'])
            self.dbg('dbg_w1pa%d' % l_, w1pad[:, 0, l_, :], [128, 128], BF16, ['w1pad', 'w1pad_v'])
        self.cg_begin('cgB2')
        for c in range(2):
            nn = 128 if c == 0 else 127
            P.op('pool', lambda e, c=c, nn=nn: e.memset(vc_aug[0:nn, c, :, 64:65], 1.0), reads=['vc_aug0'], writes=[('vc_one', c)])
            for g in range(4):
                self.load('sp', vc_aug[:, c, g, 65:129], dr['t_ovl'][c * 128:(c + 1) * 128, :], reads=['vc_aug0'], writes=[('vc_ovl', c, g)], key=('c_ovl', c, g))
        self.cg_end()
        vcr = [('vc_aug', g) for g in range(4)] + [('vc_one', c) for c in range(2)] + [('vc_ovl', c, g) for c in range(2) for g in range(4)]
        P.barrier()
        A.off = markX
        cst = [A.f32(512) for _ in range(2)]
        wv = A.bf16(8 * 512).rearrange("p (c f) -> p c f", c=8)
        wgt = A.bf16(8 * 48).rearrange("p (c f) -> p c f", c=8)
        wblk3 = [A.bf16(8 * 128).rearrange("p (c f) -> p c f", c=8) for _ in range(2)]
        Tb3 = [A.bf16(512) for _ in range(2)]
        qst = [A.bf16(512) for _ in range(2)]
        w_v = w_in.rearrange("(c p) f -> p c f", p=128)
        self.cg_begin('cgB3')
        self.load('pool', wv[:, :, 0:256], w_v[:, :, 1792:2048], reads=['w1pad', 'w1pad_v'], writes=['wv_a'], key='c_wv_a')
        self.load('pool', wv[:, :, 256:512], w_v[:, :, 2304:2560], reads=['w1pad', 'w1pad_v'], writes=['wv_b'], key='c_wv_b')
        self.load('pool', wgt, w_v[:, :, 2560:2608], reads=['w1pad', 'w1pad_v'], writes=['wgt'], key='c_wgt')
        self.cg_end()
        w_s = w_sw.rearrange("(c p) f -> p c f", p=128)
        nb = 0
        nq = 0
        deferred3 = []
        for blk in range(24):
            b = nb % 2
            nb += 1
            if blk < 16:
                natc, swc = blk * 64, blk * 64
            elif blk < 20:
                natc, swc = 1536 + (blk - 16) * 64, 1024 + (blk - 16) * 64
            else:
                natc, swc = 2048 + (blk - 20) * 64, 1280 + (blk - 20) * 64
            self.load('pool', wblk3[b][:, :, 0:64], w_v[:, :, natc:natc + 64], writes=[('wblk3', b, 0)], key=('wblk3', b, 0))
            self.load('pool', wblk3[b][:, :, 64:128], w_s[:, :, swc:swc + 64], writes=[('wblk3', b, 1)], key=('wblk3', b, 1))
            for ti in range(8):
                q = nq % 2
                nq += 1
                for c in range(8):
                    self.mm(ps[q][:, :], wblk3[b][:, c, :], x2T[:, c, ti * 512:(ti + 1) * 512], c == 0, c == 7,
                            reads=[('wblk3', b, 0), ('wblk3', b, 1), ('x2T', c)], writes=[bk(q)])
                self.load('sp', cst[q], dr['t_cs'][:, ti * 512:(ti + 1) * 512], writes=[('cst', q)], key=('cst', q))
                P.op('dve', lambda e, q=q, ti=ti: e.tensor_tensor(out=Tb3[q], in0=ps[q][:, :], in1=cst[q], op=ALU.mult),
                     reads=[bk(q), ('cst', q)], writes=[('Tb3', q)])
                def fold_part(q=q, blk=blk, ti=ti):
                    self.mm(ps[2 + q][0:64, :], fold, Tb3[q], True, True, reads=['fold', ('Tb3', q)], writes=[bk(2 + q)])
                    if blk < 16:
                        self.copy('act', qst[q][0:64, :], ps[2 + q][0:64, :], reads=[bk(2 + q)], writes=[('qst', q)])
                        self.load('act', qT_d[blk, :, ti * 512:(ti + 1) * 512], qst[q][0:64, :], reads=[('qst', q)], writes=[('qT_d', blk, ti)], key=('qst', q))
                    elif blk < 20:
                        self.copy('act', ksT[0:64, blk - 16, ti * 512:(ti + 1) * 512], ps[2 + q][0:64, :], reads=[bk(2 + q)], writes=[('ksT', blk - 16, ti)])
                    else:
                        self.copy('act', kwT[0:64, blk - 20, ti * 512:(ti + 1) * 512], ps[2 + q][0:64, :], reads=[bk(2 + q)], writes=[('kwT', blk - 20, ti)])
                deferred3.append(fold_part)
                while len(deferred3) > 1:
                    deferred3.pop(0)()
        while deferred3:
            deferred3.pop(0)()
        for n in range(NT):
            q = n % 2
            for c in range(8):
                self.mm(ps[4 + q][:, :], x2T[:, c, n * 128:(n + 1) * 128], wv[:, c, :], c == 0, c == 7,
                        reads=[('x2T', c), 'wv_a', 'wv_b'], writes=[bk(4 + q)])
            for c in range(8):
                self.mm(ps[6 + q][:, 0:48], x2T[:, c, n * 128:(n + 1) * 128], wgt[:, c, :], c == 0, c == 7,
                        reads=[('x2T', c), 'wgt'], writes=[bk(6 + q)])
            self.copy('dve', vs_aug[:, n, :, 0:64], ps[4 + q][:, 0:256].rearrange("p (g d) -> p g d", g=4), reads=[bk(4 + q)], writes=[('vs', n)])
            self.copy('dve', vw_aug[:, n, :, 0:64], ps[4 + q][:, 256:512].rearrange("p (g d) -> p g d", g=4), reads=[bk(4 + q)], writes=[('vw', n)])
            P.op('act', lambda e, n=n, q=q: e.activation(out=gates[:, n, :], in_=ps[6 + q][:, 0:48], func=AF.Sigmoid),
                 reads=[bk(6 + q)], writes=[('gates', n)])
        self.dbg('dbg_bias', biasb, [128, 2], F32, ['biasb'])
        self.dbg('dbg_kcT', kcT.rearrange("p g n -> p (g n)"), [128, 1024], BF16, [('kcT', g) for g in range(4)])
        self.dbg('dbg_vc', vc_aug.rearrange("p c g d -> p (c g d)"), [128, 1040], BF16, vcr)
        self.dbg('dbg_ksT', ksT[:, 0, 0:512], [128, 512], BF16, [('ksT', 0, 0), ('ksE', 0)])
        self.dbg('dbg_kwT', kwT[:, 1, 512:1024], [128, 512], BF16, [('kwT', 1, 1)])
        self.dbg('dbg_vs', vs_aug[:, 0:2, :, :].rearrange("p n g d -> p (n g d)"), [128, 528], BF16, [('vs', 0), ('vs', 1), 'vs_one'])
        self.dbg('dbg_gates', gates[:, 0, :], [128, 48], F32, [('gates', 0)])
        P.barrier()
        A.off = markP
        wout = A.bf16(8 * 1024).rearrange("p (c d) -> p c d", c=8)
        g_bc, b_bc = A.f32(1024), A.f32(1024)
        tB = A.bf16(S)
        trilo = A.bf16(512).rearrange("p (h t) -> p h t", h=4)
        trihi = A.bf16(512).rearrange("p (h t) -> p h t", h=4)
        Qa = [A.bf16(16 * 128).rearrange("p (h t) -> p h t", h=16) for _ in range(2)]
        PT = [A.bf16(512) for _ in range(4)]
        bonus = [A.f32(64) for _ in range(2)]
        valid = [A.f32(64) for _ in range(2)]
        negpad = [A.bf16(128) for _ in range(2)]
        ocomb = [A.bf16(1024) for _ in range(2)]
        oT = [A.bf16(1024).rearrange("p (c t) -> p c t", c=8) for _ in range(2)]
        oc = [A.f32(256).rearrange("p (h d) -> p h d", h=4) for _ in range(2)]
        otmp = [A.f32(256).rearrange("p (h d) -> p h d", h=4) for _ in range(2)]
        selb = [dict(rc=A.f32(4), imp=A.f32(64), score=A.f32(64), sc2=A.f32(64), mxa=A.f32(8), mxb=A.f32(8), sel=A.f32(64),
                     rr=A.f32(12)) for _ in range(2)]
        xtok = [A.f32(1024) for _ in range(2)]
        z = [A.f32(1024)] * 2
        xn = [A.f32(1024)] * 2
        xo = [A.f32(1024) for _ in range(2)]
        small = [dict(st=A.f32(12), mv=A.f32(2), rs=A.f32(1), nmr=A.f32(1))] * 2
        self.cg_begin('cgB4')
        self.load('sp', tB, dr['t_B'], writes=['tB'], key='c_tB')
        self.load('sp', trilo, dr['t_trilo4'].rearrange("p (h t) -> p h t", h=4), writes=['trilo'], key='c_trilo')
        self.load('sp', trihi, dr['t_trihi4'].rearrange("p (h t) -> p h t", h=4), writes=['trihi'], key='c_trihi')
        self.load('pool', wout, dr['nsa_w_out'].rearrange("(c p) d -> p c d", p=128), writes=['wout'], key='c_wout')
        self.load('sp', g_bc, dr['ln_g'][1, 0].partition_broadcast(128), writes=['g_bc'], key='c_g')
        self.load('sp', b_bc, dr['ln_b'][1, 0].partition_broadcast(128), writes=['b_bc'], key='c_b')
        self.cg_end()
        for q in range(2):
            P.op('pool', lambda e, q=q: e.memset(negpad[q], 0.0), writes=[('negpad', q)])
        zl = A.bf16(128)
        zr = A.bf16(260)
        P.op('pool', lambda e: e.memset(zl, 0.0), writes=['zl'])
        P.op('pool', lambda e: e.memset(zr, 0.0), writes=['zr'])

        def zero_acc(bank, w_):
            self.mm(ps[bank][:, 0:w_], zl, zr[:, 0:w_], True, False, reads=['zl', 'zr'], writes=[bk(bank)])
        cnt = dict(ns=0, npt=0, ng=0)
        SB = (0, 1, 7)
        NPT = len(PT)
        LAG = 2
        pending = []

        delayed = []

        def push(fn):
            pending.append(fn)
            while len(pending) > LAG:
                pending.pop(0)()
            for d_ in list(delayed):
                d_[0] -= 1
                if d_[0] <= 0:
                    delayed.remove(d_)
                    d_[1]()

        def score(lhsT, rhs, mask, lreads, qi, g):
            bq = SB[cnt['ns'] % 3]
            cnt['ns'] += 1
            S_ = ps[bq][:, :].rearrange("p (h t) -> p h t", h=4)
            self.mm(S_, lhsT, rhs, True, mask is None, reads=lreads + [('Qa', qi), ('Qneg', qi, g)], writes=[bk(bq)])
            if mask is not None:
                self.mm(S_, ident, mask[0], False, True, reads=['ident'] + mask[1], writes=[bk(bq)])
            pq = cnt['npt'] % NPT
            cnt['npt'] += 1
            P.op('act', lambda e, bq=bq, pq=pq: e.activation(out=PT[pq], in_=ps[bq][:, :], func=AF.Exp, scale=ATTN_SCALE),
                 reads=[bk(bq)], writes=[('PT', pq)])
            return pq

        def norm_branch(bank, br, i, g, gq, qi, sb_):
            sk = lambda nm: ('sel', nm, gq)
            rr = sb_['rr'].rearrange("p (b h) -> p b h", b=3)
            acc = ps[bank][:, 0:260].rearrange("p (h d) -> p h d", h=4)
            P.op('dve', lambda e: e.tensor_scalar(out=rr[:, br, :], in0=acc[:, :, 64], scalar1=1e-30, scalar2=None, op0=ALU.max),
                 reads=[bk(bank)], writes=[sk('rr%d' % br)])
            P.op('dve', lambda e: e.reciprocal(out=rr[:, br, :], in_=rr[:, br, :]), reads=[sk('rr%d' % br)], writes=[sk('rr%d' % br)])
            P.op('dve', lambda e: e.tensor_tensor(out=rr[:, br, :], in0=rr[:, br, :],
                                                  in1=gates[:, i, :].rearrange("p (h b) -> p h b", b=3)[:, 4 * g:4 * g + 4, br], op=ALU.mult),
                 reads=[sk('rr%d' % br), ('gates', i)], writes=[sk('rr%d' % br)])
            dst = oc[gq] if br == 0 else otmp[gq]
            P.op('dve', lambda e: e.tensor_tensor(out=dst, in0=acc[:, :, 0:64], in1=rr[:, br, :].unsqueeze(2).to_broadcast([128, 4, 64]), op=ALU.mult),
                 reads=[bk(bank), sk('rr%d' % br)], writes=[sk('oc') if br == 0 else sk('otmp')])
            if br == 2:
                P.op('dve', lambda e: e.tensor_tensor(out=oc[gq], in0=oc[gq], in1=otmp[gq], op=ALU.add),
                     reads=[sk('oc'), sk('otmp')], writes=[sk('oc')])
            if br == 1:
                P.op('dve', lambda e: e.tensor_tensor(out=ocomb[qi][:, g * 256:(g + 1) * 256].rearrange("p (h d) -> p h d", h=4),
                                                      in0=oc[gq], in1=otmp[gq], op=ALU.add),
                     reads=[sk('oc'), sk('otmp')], writes=[('ocomb', qi, g)])

        def selection(i, g, gq, qi, sb_):
            sk = lambda nm: ('sel', nm, gq)
            accC = ps[2][:, 0:260].rearrange("p (h d) -> p h d", h=4)
            accI = ps[3][:, 0:256].rearrange("p (h d) -> p h d", h=4)
            P.op('dve', lambda e: e.tensor_scalar(out=sb_['rc'], in0=accC[:, :, 64], scalar1=1e-30, scalar2=None, op0=ALU.max),
                 reads=[bk(2)], writes=[sk('rc')])
            P.op('dve', lambda e: e.reciprocal(out=sb_['rc'], in_=sb_['rc']), reads=[sk('rc')], writes=[sk('rc')])
            P.op('dve', lambda e: e.tensor_scalar(out=sb_['imp'], in0=accI[:, 0, :], scalar1=sb_['rc'][:, 0:1], scalar2=None, op0=ALU.mult),
                 reads=[bk(3), sk('rc')], writes=[sk('imp')])
            for h in range(1, 4):
                P.op('dve', lambda e, h=h: e.scalar_tensor_tensor(out=sb_['imp'], in0=accI[:, h, :], scalar=sb_['rc'][:, h:h + 1],
                                                                  in1=sb_['imp'], op0=ALU.mult, op1=ALU.add),
                     reads=[bk(3), sk('rc'), sk('imp')], writes=[sk('imp')])
            P.op('dve', lambda e: e.tensor_tensor(out=sb_['score'], in0=sb_['imp'], in1=bonus[qi], op=ALU.add),
                 reads=[sk('imp'), ('bonus', qi)], writes=[sk('score')])
            P.op('dve', lambda e: e.max(out=sb_['mxa'], in_=sb_['score']), reads=[sk('score')], writes=[sk('mxa')])
            P.op('dve', lambda e: e.match_replace(out=sb_['sc2'], in_to_replace=sb_['mxa'], in_values=sb_['score'], imm_value=-1e30),
                 reads=[sk('score'), sk('mxa')], writes=[sk('sc2')])
            P.op('dve', lambda e: e.max(out=sb_['mxb'], in_=sb_['sc2']), reads=[sk('sc2')], writes=[sk('mxb')])
            P.op('dve', lambda e: e.tensor_scalar(out=sb_['sel'], in0=sb_['score'], scalar1=sb_['mxb'][:, 7:8], scalar2=None, op0=ALU.is_ge),
                 reads=[sk('score'), sk('mxb')], writes=[sk('sel')])
            P.op('dve', lambda e: e.tensor_tensor(out=sb_['sel'], in0=sb_['sel'], in1=valid[qi], op=ALU.mult),
                 reads=[sk('sel'), ('valid', qi)], writes=[sk('sel')])
            P.op('dve', lambda e: e.tensor_scalar(out=negpad[gq][:, 64:128], in0=sb_['sel'], scalar1=-NEG, scalar2=NEG, op0=ALU.mult, op1=ALU.add),
                 reads=[sk('sel')], writes=[('negpad', gq)])
            norm_branch(2, 0, i, g, gq, qi, sb_)

        def tile_tail(i, qi):
            T0 = i * 128
            tb6 = ps[6][:, :].bitcast(BF16)
            for c in range(8):
                P.op('pe', lambda e, c=c: e.transpose(tb6[:, c * 128:(c + 1) * 128], ocomb[qi][:, c * 128:(c + 1) * 128], ident),
                     reads=[('ocomb', qi, c // 2), 'ident'], writes=[bk(6)])
            self.copy('act', oT[qi], tb6.rearrange("p (c t) -> p c t", c=8), reads=[bk(6)], writes=[('oT', qi)])
            for dh in range(2):
                for c in range(8):
                    self.mm(ps[6][:, :], oT[qi][:, c, :], wout[:, c, dh * 512:(dh + 1) * 512], c == 0, c == 7,
                            reads=[('oT', qi), 'wout'], writes=[bk(6)])
                P.op('dve', lambda e, dh=dh: e.scalar_tensor_tensor(out=z[qi][:, dh * 512:(dh + 1) * 512], in0=xtok[qi][:, dh * 512:(dh + 1) * 512],
                                                                    scalar=ALPHA, in1=ps[6][:, :], op0=ALU.mult, op1=ALU.add),
                     reads=[('xtok', qi), bk(6)], writes=[('z', 0)])
            tmp = dict(small[qi]); tmp['xn'] = xn[qi]
            self.ln1(z[qi], ('z', 0), tmp, 'B4ln')

            def tail_b():
                self.ln2a(z[qi], ('z', 0), tmp, 'B4ln')

                def tail_c():
                    self.ln2b(g_bc, b_bc, xo[qi], ('xo', qi), tmp, 'B4ln', mul_eng='dve')
                    self.load('sp', x3_d[T0:T0 + 128, :], xo[qi], reads=[('xo', qi)], writes=[('x3_d', i)], key=('x3st', qi))
                delayed.append([2, tail_c])
            delayed.append([2, tail_b])

        def make_group(i, g, qi, Q):
            T0 = i * 128
            gq = cnt['ng'] % 2
            cnt['ng'] += 1
            sb_ = selb[gq]
            Qg = Q[0:64, 4 * g:4 * g + 4, :]
            Qg_full = Q[:, 4 * g:4 * g + 4, :]
            nch = 1 if i < 16 else 2

            def cmp_pv(c, pq, last):
                if c == 0:
                    zero_acc(2, 260)
                    zero_acc(3, 256)
                for h in range(4):
                    self.mm(ps[2][:, h * 65:(h + 1) * 65], PT[pq][:, h * 128:(h + 1) * 128], vc_aug[:, c, g, 0:65], False, last,
                            reads=[('PT', pq)] + vcr, writes=[bk(2)])
                    self.mm(ps[3][:, h * 64:(h + 1) * 64], PT[pq][:, h * 128:(h + 1) * 128], vc_aug[:, c, g, 65:129], False, last,
                            reads=[('PT', pq)] + vcr, writes=[bk(3)])
                if last:
                    selection(i, g, gq, qi, sb_)
            def p1():
                for c in range(nch):
                    a0 = T0 - 2048 * c
                    pq = score(kcT[0:64, g, c * 128:(c + 1) * 128], Qg,
                               (tB[:, a0:a0 + 128].unsqueeze(1).to_broadcast([128, 4, 128]), ['tB']), [('kcT', g), 'kcT0'], qi, g)
                    f_ = lambda c=c, pq=pq: cmp_pv(c, pq, c == nch - 1)
                    f_._cmp = (i, g)
                    push(f_)
            wch = [m for m in range(5) if T0 - 512 + 128 * m >= 0]

            def win_pv(wi, kc0, pq, last):
                if wi == 0:
                    zero_acc(4, 260)
                for h in range(4):
                    self.mm(ps[4][:, h * 65:(h + 1) * 65], PT[pq][:, h * 128:(h + 1) * 128], vw_aug[:, kc0 // 128, g, 0:65],
                            False, last, reads=[('PT', pq), ('vw', kc0 // 128), 'vw_one'], writes=[bk(4)])
                if last:
                    norm_branch(4, 2, i, g, gq, qi, sb_)
            def emit_negmask():
                while any(getattr(f, '_cmp', None) == (i, g) for f in pending):
                    pending.pop(0)()
                tb = ps[6][:, :].bitcast(BF16)
                P.op('pe', lambda e: e.transpose(tb[:, 0:128], negpad[gq], ident), reads=[('negpad', gq), 'ident'], writes=[bk(6)])
                self.copy('act', Q[64:128, 4 * g:4 * g + 4, :], tb[64:128, 0:128].unsqueeze(1).to_broadcast([64, 4, 128]),
                          reads=[bk(6)], writes=[('Qneg', qi, g)])
            def p2():
                for wi, m in enumerate(wch):
                    kc0 = T0 - 512 + 128 * m
                    mask = None
                    if m == 0:
                        mask = (trihi, ['trihi'])
                    elif m == 4:
                        mask = (trilo, ['trilo'])
                    pq = score(kwT[0:64, g, kc0:kc0 + 128], Qg, mask, [('kwT', g, kc0 // 512)], qi, g)
                    push(lambda wi=wi, kc0=kc0, pq=pq: win_pv(wi, kc0, pq, wi == len(wch) - 1))

            def sel_pv(c, pq, last):
                if c == 0:
                    zero_acc(5, 260)
                for h in range(4):
                    self.mm(ps[5][:, h * 65:(h + 1) * 65], PT[pq][:, h * 128:(h + 1) * 128], vs_aug[:, c, g, 0:65],
                            False, last, reads=[('PT', pq), ('vs', c), 'vs_one'], writes=[bk(5)])
                if last:
                    norm_branch(5, 1, i, g, gq, qi, sb_)
                    if g == 3:
                        delayed.append([3, lambda: tile_tail(i, qi)])
            def p3():
                emit_negmask()
                for c in range(i + 1):
                    mask = (trilo, ['trilo']) if c == i else None
                    pq = score(ksT[:, g, c * 128:(c + 1) * 128], Qg_full, mask, [('ksT', g, c // 4), ('ksE', g)], qi, g)
                    push(lambda c=c, pq=pq: sel_pv(c, pq, c == i))
            return p1, p2, p3

        groups = [(i, g) for i in range(NT) for g in range(4)]
        parts = {}

        def get(k):
            if k not in parts:
                i, g = groups[k]
                qi = i % 2
                Q = Qa[qi]
                if g == 0:
                    T0 = i * 128
                    self.load('sp', Q[0:64, :, :], qT_d[:, :, T0:T0 + 128].rearrange("h d t -> d h t"), writes=[('Qa', qi)], key=('Qa', qi))
                    self.load('sp', bonus[qi], dr['t_bonus'][T0:T0 + 128, :], writes=[('bonus', qi)], key=('bonus', qi))
                    self.load('sp', valid[qi], dr['t_valid'][T0:T0 + 128, :], writes=[('valid', qi)], key=('valid', qi))
                    self.load('sp', xtok[qi], x2_d[T0:T0 + 128, :], writes=[('xtok', qi)], key=('xtok', qi))
                parts[k] = make_group(i, g, qi, Q)
            return parts[k]
        KG = len(groups)
        get(0)[0]()
        get(0)[1]()
        for k in range(KG):
            if k + 1 < KG:
                get(k + 1)[0]()
            get(k)[2]()
            if k + 1 < KG:
                get(k + 1)[1]()
            parts.pop(k, None)
        while pending:
            pending.pop(0)()
        for d_ in delayed:
            d_[1]()
        P.barrier()

    def phase_C(self, A, ps):
        P, nc, dr = self.P, self.nc, self.dram
        A.reset()
        x3_d, out_d, Xg_d, Yg_d = dr['x3_d'], dr['out'], dr['Xg_d'], dr['Yg_d']
        wgu_d, wdn_d = dr['moe_w_gu'], dr['moe_w_down']
        bk = lambda i: ('bank', i)
        NSLOT = NEXP * CAP
        slots_i = A.i32(NT * 2)
        icur = A.i32(1)
        wts = A.f32(NT * 2).rearrange("p (n k) -> p n k", k=2)
        carry = A.f32(8)
        g_bc, b_bc = A.f32(1024), A.f32(1024)
        ident32 = A.f32(128)
        identb = A.bf16(128)
        triU = A.bf16(128)
        onesb = A.bf16(128)
        ebase = A.f32(8)
        router = A.f32(64).rearrange("p (c e) -> p c e", c=8)
        self.cg_begin('cgC')
        self.load('sp', g_bc, dr['ln_g'][1, 1].partition_broadcast(128), writes=['g_bc'], key='c_g')
        self.load('sp', b_bc, dr['ln_b'][1, 1].partition_broadcast(128), writes=['b_bc'], key='c_b')
        self.load('sp', ident32, dr['t_ident32'], writes=['ident32'], key='c_i32')
        self.load('sp', identb, dr['t_ident'], writes=['identb'], key='c_ident')
        self.load('sp', triU, dr['t_triU'], writes=['triU'], key='c_triU')
        self.load('sp', onesb, dr['t_ones'], writes=['onesb'], key='c_ones')
        self.load('sp', ebase, dr['t_ebase'][0].partition_broadcast(128), writes=['ebase'], key='c_ebase')
        self.load('sp', router, dr['moe_router'].rearrange("(c p) e -> p c e", p=128), writes=['router'], key='c_router')
        self.cg_end()
        P.op('dve', lambda e: e.memset(carry, 0.0), writes=['carry'])
        mark2 = A.off
        xt = [A.f32(1024) for _ in range(3)]
        xb_all = A.bf16(NT * 1024).rearrange("p (n d) -> p n d", n=NT)
        xT32 = [A.f32(1024).rearrange("p (c t) -> p c t", c=8) for _ in range(3)]
        sm = [dict(lg=A.f32(8), mx=A.f32(8), oh1=A.f32(8), oh2=A.f32(8), mk=A.bf16(8), pos=A.f32(8), sv=A.f32(8),
                   tmp=A.f32(8), s12=A.f32(2), dd=A.f32(2)) for _ in range(3)]
        zt = xT32[0].rearrange("p c t -> p (c t)").bitcast(BF16).rearrange("p (a d) -> p a d", a=2)
        P.op('pool', lambda e: e.memset(zt, 0.0), writes=[('xT32a', 0), ('xT32b', 0)])
        self.load('sp', Yg_d[NSLOT:NSLOT + 1, :], zt[0:1, :, :].rearrange("p a d -> p (a d)"), reads=[('xT32a', 0), ('xT32b', 0)],
                  writes=['Yg_zero'], key='ygz')
        for r0 in range(0, NSLOT, 256):
            self.load('sp', Xg_d[r0:r0 + 256, :].rearrange("(a p) d -> p a d", p=128), zt, reads=[('xT32a', 0), ('xT32b', 0)],
                      writes=['Xg_zero'], key='xgz')
        def stageA(n):
            q, qb, t0 = n % 3, n % 2, n * 128
            k_ = lambda name: (name, q)
            self.load('sp', xt[q], x3_d[t0:t0 + 128, :], writes=[k_('xt')], key=('xt', q))
            self.copy('act', xb_all[:, n, :], xt[q], reads=[k_('xt')], writes=[('xb', n)])
            pa, pb = ps[2 * qb], ps[2 * qb + 1]
            for c in range(8):
                pt = pa if c < 4 else pb
                self.mm(pt[:, (c % 4) * 128:(c % 4) * 128 + 128], xt[q][:, c * 128:(c + 1) * 128], ident32, True, True,
                        reads=[k_('xt'), 'ident32'], writes=[bk(2 * qb + (0 if c < 4 else 1))])
            self.copy('act', xT32[q][:, 0:4, :], pa[:, :].rearrange("p (c t) -> p c t", c=4), reads=[bk(2 * qb)], writes=[k_('xT32a')])
            self.copy('dve', xT32[q][:, 4:8, :], pb[:, :].rearrange("p (c t) -> p c t", c=4), reads=[bk(2 * qb + 1)], writes=[k_('xT32b')])
            pl = ps[4 + qb]
            for c in range(8):
                self.mm(pl[:, 0:8], xT32[q][:, c, :], router[:, c, :], c == 0, c == 7,
                        reads=[k_('xT32a'), k_('xT32b'), 'router'], writes=[bk(4 + qb)])

        def stageB(n):
            q, qb = n % 3, n % 2
            m = sm[q]
            k_ = lambda name: (name, q)
            pl = ps[4 + qb]
            self.copy('dve', m['lg'], pl[:, 0:8], reads=[bk(4 + qb)], writes=[k_('lg')])
            P.op('dve', lambda e: e.max(out=m['mx'], in_=m['lg']), reads=[k_('lg')], writes=[k_('mx')])
            P.op('dve', lambda e: e.tensor_scalar(out=m['oh1'], in0=m['lg'], scalar1=m['mx'][:, 0:1], scalar2=None, op0=ALU.is_equal),
                 reads=[k_('lg'), k_('mx')], writes=[k_('oh1')])
            P.op('dve', lambda e: e.tensor_scalar(out=m['oh2'], in0=m['lg'], scalar1=m['mx'][:, 1:2], scalar2=None, op0=ALU.is_equal),
                 reads=[k_('lg'), k_('mx')], writes=[k_('oh2')])
            P.op('dve', lambda e: e.tensor_tensor(out=m['dd'][:, 0:1], in0=m['mx'][:, 0:1], in1=m['mx'][:, 1:2], op=ALU.subtract),
                 reads=[k_('mx')], writes=[k_('dd')])
            P.op('act', lambda e: e.activation(out=wts[:, n, 0:1], in_=m['dd'][:, 0:1], func=AF.Sigmoid),
                 reads=[k_('dd')], writes=[('wts', n)])
            P.op('act', lambda e: e.activation(out=wts[:, n, 1:2], in_=m['dd'][:, 0:1], func=AF.Sigmoid, scale=-1.0),
                 reads=[k_('dd')], writes=[('wts', n)])
            P.op('dve', lambda e: e.tensor_tensor(out=m['mk'], in0=m['oh1'], in1=m['oh2'], op=ALU.add),
                 reads=[k_('oh1'), k_('oh2')], writes=[k_('mk')])
            pp = ps[6 + qb]
            self.mm(pp[:, 0:8], triU, m['mk'], True, True, reads=['triU', k_('mk')], writes=[bk(6 + qb)])
            self.mm(pp[:, 8:16], onesb, m['mk'], True, True, reads=['onesb', k_('mk')], writes=[bk(6 + qb)])

        def stageC(n):
            q, qb = n % 3, n % 2
            m = sm[q]
            k_ = lambda name: (name, q)
            pp = ps[6 + qb]
            P.op('dve', lambda e: e.tensor_tensor(out=m['pos'], in0=pp[:, 0:8], in1=carry, op=ALU.add),
                 reads=[bk(6 + qb), 'carry'], writes=[k_('pos')])
            P.op('dve', lambda e: e.tensor_tensor(out=carry, in0=pp[:, 8:16], in1=carry, op=ALU.add),
                 reads=[bk(6 + qb), 'carry'], writes=['carry'])
            P.op('dve', lambda e: e.tensor_scalar(out=m['tmp'], in0=m['pos'], scalar1=float(CAP), scalar2=1.0e6, op0=ALU.is_ge, op1=ALU.mult),
                 reads=[k_('pos')], writes=[k_('tmp')])
            P.op('dve', lambda e: e.tensor_tensor(out=m['sv'], in0=m['pos'], in1=ebase, op=ALU.add),
                 reads=[k_('pos'), 'ebase'], writes=[k_('sv')])
            P.op('dve', lambda e: e.tensor_tensor(out=m['sv'], in0=m['sv'], in1=m['tmp'], op=ALU.add),
                 reads=[k_('sv'), k_('tmp')], writes=[k_('sv')])
            P.op('dve', lambda e: e.tensor_scalar(out=m['sv'], in0=m['sv'], scalar1=float(NSLOT), scalar2=None, op0=ALU.min),
                 reads=[k_('sv')], writes=[k_('sv')])
            for w_, oh in enumerate(('oh1', 'oh2')):
                P.op('dve', lambda e, oh=oh: e.tensor_tensor(out=m['tmp'], in0=m[oh], in1=m['sv'], op=ALU.mult),
                     reads=[k_(oh), k_('sv')], writes=[k_('tmp')])
                P.op('dve', lambda e, w_=w_: e.reduce_sum(out=m['s12'][:, w_:w_ + 1], in_=m['tmp'], axis=mybir.AxisListType.X),
                     reads=[k_('tmp')], writes=[k_('s12')])
            P.op('dve', lambda e: e.tensor_copy(out=slots_i[:, 2 * n:2 * n + 2], in_=m['s12'][:, 0:2]), reads=[k_('s12')], writes=[('slots', n)])

        for n in range(NT + 2):
            if n < NT:
                stageA(n)
            if 0 <= n - 1 < NT:
                stageB(n - 1)
            if 0 <= n - 2 < NT:
                stageC(n - 2)

        def scat_fn(g, sem):
            base = self.loop_cnt
            with g.Fori(0, 2 * NT) as i:
                g.tensor_copy(out=icur, in_=slots_i[:, bass.ts(i, 1)]).then_inc(self.loop_sem, 1)
                g.wait_ge(self.loop_sem, base + i + 1)
                g.indirect_dma_start(out=Xg_d, out_offset=bass.IndirectOffsetOnAxis(ap=icur, axis=0),
                                     in_=xb_all[:, i // 2, :], in_offset=None,
                                     bounds_check=NSLOT - 1, oob_is_err=False).then_inc(sem, 16)
                g.wait_ge(sem, 16 * i + 16)
            self.loop_cnt += 2 * NT
        P.dma('pool', scat_fn, reads=[('xb', n) for n in range(NT)] + [('slots', n) for n in range(NT)] + ['Xg_zero'],
              writes=['Xg_d', 'icur', 'Xg_zero'], key='scatloop', count=2 * NT, raw=True)
        P.barrier()
        A.off = mark2
        TT = [(0, 512), (512, 512), (1024, CAP - 1024)]
        XgT = [A.bf16(8 * CAP).rearrange("p (c t) -> p c t", c=8) for _ in range(2)]
        wd = [A.bf16(14 * 1024).rearrange("p (j d) -> p j d", j=14) for _ in range(2)]
        wg = [A.bf16(8 * 256).rearrange("p (c f) -> p c f", c=8) for _ in range(3)]
        wu = [A.bf16(8 * 256).rearrange("p (c f) -> p c f", c=8) for _ in range(3)]
        actT = A.bf16(14 * CAP).rearrange("p (j t) -> p j t", j=14)
        xg = [A.bf16(1024) for _ in range(2)]
        ysb = [A.bf16(1024) for _ in range(2)]
        sg = [A.f32(512) for _ in range(2)]
        cnts = dict(blk=0, gu=0, dn=0, xg=0)
        Ygv = Yg_d[0:NSLOT, :].rearrange("s (h d) -> s h d", h=2)

        def load_xgT(e_):
            X = XgT[e_ % 2]
            for s_ in range(CAP // 128):
                q = cnts['xg'] % 2
                cnts['xg'] += 1
                self.load('sp', xg[q], Xg_d[e_ * CAP + s_ * 128:e_ * CAP + (s_ + 1) * 128, :], writes=[('xg', q)], key=('xg', q))
                tb = ps[4 + q][:, :].bitcast(BF16)
                for c in range(8):
                    P.op('pe', lambda e, c=c, q=q, tb=tb: e.transpose(tb[:, c * 128:(c + 1) * 128], xg[q][:, c * 128:(c + 1) * 128], identb),
                         reads=[('xg', q), 'identb'], writes=[bk(4 + q)])
                self.copy(self.evac_eng(), X[:, :, s_ * 128:(s_ + 1) * 128], tb.rearrange("p (c t) -> p c t", c=8),
                          reads=[bk(4 + q)], writes=[('XgT', e_ % 2, s_)])

        load_xgT(0)
        for u in range(2 * NEXP):
            e_, half = u // 2, u % 2
            X = XgT[e_ % 2]
            W = wd[u % 2]
            f0 = half * (DFE // 2)
            self.load('pool', W, wdn_d[e_, f0:f0 + DFE // 2, :].rearrange("(j p) d -> p j d", p=128), writes=[('wd', u % 2)], key=('wd', u % 2))
            wv = wgu_d[e_].rearrange("(c p) f -> p c f", p=128)
            for fb in range(7):
                b = cnts['blk'] % 3
                cnts['blk'] += 1
                self.load('pool', wg[b], wv[:, :, f0 + fb * 256:f0 + fb * 256 + 256], writes=[('wg', b)], key=('wg', b))
                self.load('pool', wu[b], wv[:, :, DFE + f0 + fb * 256:DFE + f0 + fb * 256 + 256], writes=[('wu', b)], key=('wu', b))
                for jj in range(2):
                    j = fb * 2 + jj
                    for ti, (ta, tn) in enumerate(TT):
                        q = cnts['gu'] % 2
                        cnts['gu'] += 1
                        pg, pu = ps[2 * q], ps[2 * q + 1]
                        xr = [('XgT', e_ % 2, s_) for s_ in range(ta // 128, (ta + tn) // 128)]
                        for kc in range(8):
                            self.mm(pg[:, 0:tn], wg[b][:, kc, jj * 128:(jj + 1) * 128], X[:, kc, ta:ta + tn],
                                    kc == 0, kc == 7, reads=[('wg', b)] + xr, writes=[bk(2 * q)])
                        for kc in range(8):
                            self.mm(pu[:, 0:tn], wu[b][:, kc, jj * 128:(jj + 1) * 128], X[:, kc, ta:ta + tn],
                                    kc == 0, kc == 7, reads=[('wu', b)] + xr, writes=[bk(2 * q + 1)])
                        P.op('act', lambda e, q=q, pg=pg, tn=tn: e.activation(out=sg[q][:, 0:tn], in_=pg[:, 0:tn], func=AF.Silu),
                             reads=[bk(2 * q)], writes=[('sg', q)])
                        P.op('dve', lambda e, q=q, pu=pu, j=j, ta=ta, tn=tn: e.tensor_tensor(
                            out=actT[:, j, ta:ta + tn], in0=sg[q][:, 0:tn], in1=pu[:, 0:tn], op=ALU.mult),
                            reads=[('sg', q), bk(2 * q + 1)], writes=[('actT', j, ti)])
            if half == 1 and e_ + 1 < NEXP:
                load_xgT(e_ + 1)
            for s_ in range(CAP // 128):
                q = cnts['dn'] % 2
                cnts['dn'] += 1
                ti = min(s_ // 4, 2)
                pa, pb = ps[4 + 2 * q], ps[5 + 2 * q]
                for dh, pt in enumerate((pa, pb)):
                    for j in range(14):
                        self.mm(pt[:, :], actT[:, j, s_ * 128:(s_ + 1) * 128], W[:, j, dh * 512:(dh + 1) * 512],
                                j == 0, j == 13, reads=[('actT', j, ti), ('wd', u % 2)], writes=[bk(4 + 2 * q + dh)])
                self.copy('act', ysb[q][:, 0:512], pa[:, :], reads=[bk(4 + 2 * q)], writes=[('ysb', q)])
                self.copy('dve', ysb[q][:, 512:1024], pb[:, :], reads=[bk(5 + 2 * q)], writes=[('ysb', q)])
                r0 = e_ * CAP + s_ * 128
                self.load('sp', Ygv[r0:r0 + 128, half, :], ysb[q], reads=[('ysb', q)], writes=[('Yg_d', u, s_)], key=('yst', q))
        P.barrier()
        A.off = mark2
        HT = NT // 2
        ogh = A.bf16(HT * 2 * 2048).rearrange("p (k d) -> p k d", k=HT * 2)
        xt = [A.f32(1024) for _ in range(3)]
        tsum = [[A.f32(1024) for _ in range(2)] for _ in range(3)]
        z = [A.f32(1024) for _ in range(3)]
        xn = z
        xo = [A.f32(1024) for _ in range(3)]
        small = [dict(st=A.f32(12), mv=A.f32(2), rs=A.f32(1), nmr=A.f32(1)) for _ in range(3)]
        defCA, defCB = [], []
        for n in range(NT):
            q = n % 3
            t0 = n * 128
            H = n // HT
            if defCB:
                defCB.pop(0)()
            if defCA:
                defCA.pop(0)()
            if n % HT == 0:
                def gat_fn(g, sem, H=H):
                    base = self.loop_cnt
                    with g.Fori(0, 2 * HT) as i:
                        g.tensor_copy(out=icur, in_=slots_i[:, bass.ts(i + 2 * HT * H, 1)]).then_inc(self.loop_sem, 1)
                        g.wait_ge(self.loop_sem, base + i + 1)
                        g.indirect_dma_start(out=ogh[:, i, :], out_offset=None, in_=Yg_d,
                                             in_offset=bass.IndirectOffsetOnAxis(ap=icur, axis=0),
                                             bounds_check=NSLOT, oob_is_err=False).then_inc(sem, 16)
                        g.wait_ge(sem, 16 * i + 16)
                    self.loop_cnt += 2 * HT
                P.dma('pool', gat_fn, reads=['icur'], writes=['ogh', 'icur'], key=('gatloop', H), count=2 * HT, raw=True)
            self.load('sp', xt[q], x3_d[t0:t0 + 128, :], writes=[('xt', q)], key=('xt', q))
            P.op('act', lambda e, q=q: e.activation(out=z[q], in_=xt[q], func=AF.Copy, scale=ALPHA), reads=[('xt', q)], writes=[('z', q)])
            r = 2 * (n % HT)
            t1, t2 = tsum[q][0], tsum[q][1]
            P.op('dve', lambda e, r=r, t1=t1: e.tensor_tensor(out=t1, in0=ogh[:, r, 0:1024], in1=ogh[:, r, 1024:2048], op=ALU.add),
                 reads=['ogh'], writes=[('t1', q)])
            P.op('dve', lambda e, r=r, t2=t2: e.tensor_tensor(out=t2, in0=ogh[:, r + 1, 0:1024], in1=ogh[:, r + 1, 1024:2048], op=ALU.add),
                 reads=['ogh'], writes=[('t2', q)])
            P.op('dve', lambda e, q=q, n=n, t1=t1: e.scalar_tensor_tensor(out=z[q], in0=t1, scalar=wts[:, n, 0:1], in1=z[q], op0=ALU.mult, op1=ALU.add),
                 reads=[('t1', q), ('z', q)], writes=[('z', q)])
            P.op('dve', lambda e, q=q, n=n, t2=t2: e.scalar_tensor_tensor(out=z[q], in0=t2, scalar=wts[:, n, 1:2], in1=z[q], op0=ALU.mult, op1=ALU.add),
                 reads=[('t2', q), ('z', q)], writes=[('z', q)])
            tmp = dict(small[q]); tmp['xn'] = xn[q]
            self.ln1(z[q], ('z', q), tmp, 'C3ln%d' % q)

            def pieceA(q=q, n=n, t0=t0, tmp=tmp):
                self.ln2a(z[q], ('z', q), tmp, 'C3ln%d' % q)

                def pieceB():
                    self.ln2b(g_bc, b_bc, xo[q], ('xo', q), tmp, 'C3ln%d' % q, mul_eng='dve')
                    self.load('act', out_d[t0:t0 + 128, :], xo[q], reads=[('xo', q)], writes=[('out', n)], key=('ost', q))
                defCB.append(pieceB)
            defCA.append(pieceA)
        while defCA or defCB:
            if defCB:
                defCB.pop(0)()
            if defCA:
                defCA.pop(0)()
        P.barrier()


def make_tables():
    import ml_dtypes
    bf = ml_dtypes.bfloat16
    t = {}
    inv = np.zeros((4, 512), np.float32)
    for g in range(4):
        w = 2 ** (g + 1)
        inv[g] = 1.0 / np.minimum(np.arange(512) + 1, w)
    t['t_invc'] = inv.reshape(1, 4 * 512)
    t['t_ident'] = np.eye(128, dtype=np.float32).astype(bf)
    t['t_ident32'] = np.eye(128, dtype=np.float32)
    t['t_triU'] = np.triu(np.ones((128, 128), np.float32), 1).astype(bf)
    t['t_ones'] = np.ones((128, 128), np.float32).astype(bf)
    t['t_ebase'] = (np.arange(8, dtype=np.float32) * CAP).reshape(1, 8)
    half = 32
    freqs = np.power(np.float32(10000.0), -np.arange(half, dtype=np.float32) / np.float32(half)).astype(np.float32)
    def cs_table(pos):
        ang = pos.astype(np.float32)[None, :] * freqs[:, None]
        cos = np.cos(ang).astype(np.float32); sin = np.sin(ang).astype(np.float32)
        return np.concatenate([cos, cos, -sin, sin], axis=0)
    t['t_cs'] = cs_table(np.arange(S))
    csc = np.zeros((128, 256), np.float32)
    csc[:, :255] = cs_table(16 * np.arange(255) + 31)
    t['t_csc'] = csc
    p = np.arange(128)[:, None]; u = np.arange(S)[None, :]
    t['t_B'] = np.where(16 * p + 31 <= u, 0.0, NEG).astype(np.float32).astype(bf)
    n_ = np.arange(128)[:, None]; t_ = np.arange(128)[None, :]
    lo = np.where(n_ <= t_, 0.0, NEG).astype(np.float32)
    hi = np.where(n_ > t_, 0.0, NEG).astype(np.float32)
    t['t_trilo4'] = np.tile(lo, (1, 4)).astype(bf)
    t['t_trihi4'] = np.tile(hi, (1, 4)).astype(bf)
    t['t_E'] = (np.arange(S)[None, :] // 64 == np.arange(64)[:, None]).astype(np.float32).astype(bf)
    fold = np.zeros((128, 64), np.float32); fold[np.arange(64), np.arange(64)] = 1; fold[np.arange(64) + 64, np.arange(64)] = 1
    t['t_fold'] = fold.astype(bf)
    tt = np.arange(S)[:, None]; jj = np.arange(64)[None, :]
    blk_t = tt // 64
    bvalid = jj <= blk_t
    forced = (jj == 0) | (jj == blk_t) | (jj == blk_t - 1)
    t['t_bonus'] = np.where(bvalid, 1000.0 * forced, -1.0).astype(np.float32)
    t['t_valid'] = bvalid.astype(np.float32)
    cstart = 16 * np.arange(255)[:, None]; sstart = 64 * np.arange(64)[None, :]
    ovl = np.zeros((256, 64), np.float32)
    ovl[:255] = ((cstart <= sstart + 63) & (cstart + 31 >= sstart)).astype(np.float32)
    t['t_ovl'] = ovl.astype(bf)
    return t


TABLE_SPECS = {
    't_invc': ([1, 2048], F32),
    't_ident': ([128, 128], BF16),
    't_ident32': ([128, 128], F32),
    't_triU': ([128, 128], BF16),
    't_ones': ([128, 128], BF16),
    't_ebase': ([1, 8], F32),
    't_cs': ([128, S], F32), 't_csc': ([128, 256], F32), 't_B': ([128, S], BF16),
    't_trilo4': ([128, 512], BF16), 't_trihi4': ([128, 512], BF16), 't_E': ([64, S], BF16), 't_fold': ([128, 64], BF16),
    't_bonus': ([S, 64], F32), 't_valid': ([S, 64], F32), 't_ovl': ([256, 64], BF16),
}

PHASE_INPUTS = {
    'A1': ['x', 'xT', 'pool_w', 'pool_scale', 'ln_g', 'ln_b', 't_invc', 't_ident'],
    'A2': ['ffn_w_gu', 'ffn_w_down', 'ln_g', 'ln_b', 't_ident'],
    'B': ['nsa_w_in', 'w_in_sw', 'nsa_pe_k', 'nsa_w1_k', 'nsa_w2_k', 'w2k_sw', 'nsa_pe_v', 'nsa_w1_v', 'nsa_w2_v', 'nsa_w_out', 'ln_g', 'ln_b',
          't_ident', 't_cs', 't_csc', 't_B', 't_trilo4', 't_trihi4', 't_E', 't_fold', 't_bonus', 't_valid', 't_ovl'],
    'C': ['moe_router', 'moe_w_gu', 'moe_w_down', 'ln_g', 'ln_b', 't_ident', 't_ident32', 't_triU', 't_ones', 't_ebase'],
}
INPUT_SPECS = {
    'x': ([S, D], F32), 'xT': ([8, 128, S], F32),
    'pool_w': ([4, 256, 256], F32), 'pool_scale': ([1, D], F32),
    'ln_g': ([2, 2, D], F32), 'ln_b': ([2, 2, D], F32),
    'ffn_w_gu': ([D, 2 * D_FF], F32), 'ffn_w_down': ([D_FF, D], F32),
    'nsa_w_in': ([D, IN_PROJ], F32), 'w_in_sw': ([D, 1536], F32), 'nsa_pe_k': ([32, 64], F32), 'nsa_w1_k': ([2048, 128], F32),
    'nsa_w2_k': ([128, 64], F32), 'w2k_sw': ([128, 64], F32), 'nsa_pe_v': ([32, 64], F32), 'nsa_w1_v': ([2048, 128], F32),
    'nsa_w2_v': ([128, 64], F32), 'nsa_w_out': ([D, D], F32),
    'moe_router': ([D, NEXP], F32), 'moe_w_gu': ([NEXP, D, 2 * DFE], F32), 'moe_w_down': ([NEXP, DFE, D], F32),
}
INPUT_SPECS.update(TABLE_SPECS)
MID = {
    'x1_d': ([S, D], F32, 'A1', ['A2']),
    'x1T_d': ([8, 128, S], BF16, 'A1', ['A2']),
    'x2_d': ([S, D], F32, 'A2', ['B']),
    'x2T_d': ([8, 128, S], BF16, 'A2', ['B']),
    'x3_d': ([S, D], F32, 'B', ['C']),
    'qT_d': ([16, 64, S], BF16, 'B', ['B']),
    'Xg_d': ([NEXP * CAP, D], BF16, 'C', ['C']),
    'Yg_d': ([NEXP * CAP + 1, 2 * D], BF16, 'C', ['C']),
    'out': ([S, D], F32, 'C', []),
}
ARENA_COLS = 51 * 1024 + 512


def swap_halves_cols(w, head=64):
    r, c = w.shape
    return np.ascontiguousarray(w.reshape(r, c // head, 2, head // 2)[:, :, ::-1, :].reshape(r, c))


def host_inputs(inputs, b, tabs):
    x = np.asarray(inputs['x'][b], dtype=np.float32)
    w_in = np.asarray(inputs['nsa_w_in'][0])
    d = {
        'x': np.ascontiguousarray(x),
        'xT': np.ascontiguousarray(x.T.reshape(8, 128, S)),
        'pool_w': np.asarray(inputs['pool_w'][0]), 'pool_scale': np.asarray(inputs['pool_scale']),
        'ln_g': np.asarray(inputs['ln_g']), 'ln_b': np.asarray(inputs['ln_b']),
        'ffn_w_gu': np.asarray(inputs['ffn_w_gu'][0]), 'ffn_w_down': np.asarray(inputs['ffn_w_down'][0]),
        'nsa_w_in': w_in,
        'w_in_sw': swap_halves_cols(np.concatenate([w_in[:, 0:1024], w_in[:, 1536:1792], w_in[:, 2048:2304]], axis=1)),
        'nsa_pe_k': np.asarray(inputs['nsa_pe_k'][0]), 'nsa_w1_k': np.asarray(inputs['nsa_w1_k'][0]),
        'nsa_w2_k': np.asarray(inputs['nsa_w2_k'][0]), 'w2k_sw': swap_halves_cols(np.asarray(inputs['nsa_w2_k'][0])),
        'nsa_pe_v': np.asarray(inputs['nsa_pe_v'][0]), 'nsa_w1_v': np.asarray(inputs['nsa_w1_v'][0]),
        'nsa_w2_v': np.asarray(inputs['nsa_w2_v'][0]), 'nsa_w_out': np.asarray(inputs['nsa_w_out'][0]),
        'moe_router': np.asarray(inputs['moe_router'][0]), 'moe_w_gu': np.asarray(inputs['moe_w_gu'][0]),
        'moe_w_down': np.asarray(inputs['moe_w_down'][0]),
    }
    d.update(tabs)
    return d


def build(phases, dump=(), debug=False):
    nc = bass.Bass("TRN2", target_bir_lowering=False)
    k = K(nc, phases, None)
    k.debug = debug
    need = []
    for ph in phases:
        for n in PHASE_INPUTS[ph]:
            if n not in need:
                need.append(n)
    for n in need:
        shp, dt = INPUT_SPECS[n]
        k.din(n, shp, dt)
    outs = []
    ins = list(need)
    for n, (shp, dt, prod, cons) in MID.items():
        p_here = prod in phases
        c_here = any(c in phases for c in cons)
        if not (p_here or c_here):
            continue
        if n in dump and p_here:
            c_here_eff = False
        else:
            c_here_eff = c_here
        if p_here and c_here and n in dump:
            t = nc.dram_tensor(n, list(shp), dt, kind="ExternalOutput")
            k.dram[n] = t.ap()
            outs.append(n)
        else:
            k.dmid(n, shp, dt, p_here, c_here_eff)
            if p_here and not c_here_eff:
                outs.append(n)
            elif c_here and not p_here:
                ins.append(n)
    with contextlib.ExitStack() as es:
        big = es.enter_context(nc.sbuf_tensor("arena", [128, ARENA_COLS], F32))
        A = Arena(big, ARENA_COLS)
        ps = [es.enter_context(nc.psum_tensor("ps%d" % i, [128, 512], F32)) for i in range(8)]
        k.loop_sem = es.enter_context(nc.semaphore('loopc'))
        k.loop_cnt = 0
        for ph in phases:
            getattr(k, 'phase_' + ph)(A, ps)
        k.P.emit()
    outs = outs + k.dbg_outs
    return nc, ins, outs


PHASES = ['A1', 'A2', 'B', 'C']
_CACHE = {}


def kernel(**inputs):
    tabs = make_tables()
    if 'nc' not in _CACHE:
        _CACHE['nc'] = build(PHASES)
    nc, ins, outs = _CACHE['nc']
    B = inputs['x'].shape[0]
    in_maps = []
    for b in range(B):
        hi = host_inputs(inputs, b, tabs)
        in_maps.append({n: hi[n] for n in ins})
    res = run_bass_kernel_spmd(nc, in_maps, core_ids=list(range(B)))
    out = np.stack([np.asarray(r['out'], dtype=np.float32) for r in res.results], axis=0)
    return out
```
